# Optimizing a Trainium2 kernel written in Bass

```python
import jax, jax.numpy as jnp
from jax import lax
import numpy as np

D_MODEL = 2048
BATCH = 4
SEQ = 2048
DEPTH = 1
DEC_BATCH = 32
DEC_SEQ = 8
PAST_LEN = 8192
PAGE_SIZE = 128

HEAD_DIM = 64
ATT_SLOTS = 16
DIL_PATTERNS = ((128, 1), (512, 4), (2048, 16))
N_GROUPS = len(DIL_PATTERNS)
ATT_WIDTH = ATT_SLOTS * HEAD_DIM
QKV_WIDTH = N_GROUPS * ATT_SLOTS * HEAD_DIM
CONV_WIDTH = D_MODEL - ATT_WIDTH
CONV_K = 3
IN_WIDTH = 3 * QKV_WIDTH + 3 * CONV_WIDTH
N_KEYS = 128
N_EXPERTS = N_KEYS * N_KEYS
PEER_HEADS = 8
PEER_QDIM = 256
PEER_HALF = PEER_QDIM // 2
PEER_TOPK = 16
PEER_BLOCK = 128
NORM_EPS = 1e-6
NEG_INF = -1e30

kernel_name = 'hymba_dilated_attn_shortconv_peer_step'


def rmsnorm(x, g):
    xf = x.astype(jnp.float32)
    y = xf * lax.rsqrt(jnp.mean(xf * xf, axis=-1, keepdims=True) + NORM_EPS)
    return (y * g.astype(jnp.float32)).astype(x.dtype)


def alibi_slopes():
    n = N_GROUPS * ATT_SLOTS
    i = jnp.arange(1, n + 1, dtype=jnp.float32)
    return jnp.exp2(-8.0 * i / n).reshape(N_GROUPS, ATT_SLOTS)


def split_projection(x, norm_g, w_in):
    xn = rmsnorm(x, norm_g)
    p = xn @ w_in
    cuts = [QKV_WIDTH, 2 * QKV_WIDTH, 3 * QKV_WIDTH,
            3 * QKV_WIDTH + CONV_WIDTH, 3 * QKV_WIDTH + 2 * CONV_WIDTH]
    q, k, v, gate_b, gate_c, h = jnp.split(p, cuts, axis=-1)
    heads = x.shape[:2] + (N_GROUPS, ATT_SLOTS, HEAD_DIM)
    return q.reshape(heads), k.reshape(heads), v.reshape(heads), gate_b, gate_c * h


def causal_conv(up, w, n_out):
    y = up[:, 0:n_out] * w[0]
    for j in range(1, CONV_K):
        y = y + up[:, j:j + n_out] * w[j]
    return y


def to_sub(a, dil, blk, seq_pad):
    b, s = a.shape[:2]
    rest = a.shape[2:]
    a = jnp.pad(a, ((0, 0), (0, seq_pad - s)) + ((0, 0),) * len(rest))
    a = a.reshape((b, seq_pad // dil, dil) + rest)
    a = jnp.moveaxis(a, 2, 1)
    return a.reshape((b, dil, seq_pad // (dil * blk), blk) + rest)


def from_sub(a, dil, seq):
    b = a.shape[0]
    rest = a.shape[4:]
    a = a.reshape((b, dil, -1) + rest)
    a = jnp.moveaxis(a, 1, 2)
    return a.reshape((b, -1) + rest)[:, :seq]


def dilated_prompt(q, k, v, window, dil, slopes):
    b, s, h, dh = q.shape
    n_back = window // dil
    blk = n_back
    span = dil * blk
    seq_pad = -(-s // span) * span
    qb, kb, vb = (to_sub(a, dil, blk, seq_pad) for a in (q, k, v))
    nb = seq_pad // span

    def with_prev(a):
        prev = jnp.pad(a, ((0, 0), (0, 0), (1, 0), (0, 0), (0, 0), (0, 0)))[:, :, :-1]
        return jnp.concatenate([prev, a], axis=3)

    kk, vv = with_prev(kb), with_prev(vb)
    blocks = jnp.arange(nb)[:, None, None]
    qi = blocks * blk + jnp.arange(blk)[None, :, None]
    kj = (blocks - 1) * blk + jnp.arange(2 * blk)[None, None, :]
    steps = qi - kj
    valid = (steps >= 0) & (steps <= n_back) & (kj >= 0)
    dist = (steps * dil).astype(jnp.float32)
    bias = jnp.where(valid[:, None], -slopes[None, :, None, None] * dist[:, None], NEG_INF)
    scale = HEAD_DIM ** -0.5
    sc = jnp.einsum('brnqhd,brnkhd->brnhqk', qb, kk).astype(jnp.float32) * scale + bias
    lse = jax.nn.logsumexp(sc, axis=-1)
    p = jnp.exp(sc - lse[..., None])
    o = jnp.einsum('brnhqk,brnkhd->brnqhd', p, vv.astype(jnp.float32))
    o = from_sub(o, dil, s).astype(q.dtype)
    lse = from_sub(jnp.moveaxis(lse, 3, 4), dil, s)
    return o, lse


def dilated_sample(q, k_all, v_all, window, dil, slopes, n_prev):
    t = q.shape[1]
    n_back = window // dil
    steps = jnp.arange(n_back + 1)
    idx = n_prev + jnp.arange(t)[:, None] - steps[None, :] * dil
    valid = idx >= 0
    idx_c = jnp.maximum(idx, 0)
    kg = k_all[:, idx_c]
    vg = v_all[:, idx_c]
    dist = (steps * dil).astype(jnp.float32)
    bias = jnp.where(valid[None], -slopes[:, None, None] * dist[None, None, :], NEG_INF)
    scale = HEAD_DIM ** -0.5
    sc = jnp.einsum('bthd,btkhd->bhtk', q, kg).astype(jnp.float32) * scale + bias
    lse = jax.nn.logsumexp(sc, axis=-1)
    p = jnp.exp(sc - lse[..., None])
    o = jnp.einsum('bhtk,btkhd->bthd', p, vg.astype(jnp.float32)).astype(q.dtype)
    return o, jnp.transpose(lse, (0, 2, 1))


def combine_groups(outs, lses):
    o = jnp.stack(outs, 0).astype(jnp.float32)
    w = jax.nn.softmax(jnp.stack(lses, 0), axis=0)
    return jnp.einsum('gnsh,gnshd->nshd', w, o).astype(outs[0].dtype)


def mixer_output(att, conv_y, g_att, g_conv, w_out):
    n, s = att.shape[:2]
    cat = jnp.concatenate([rmsnorm(att.reshape(n, s, ATT_WIDTH), g_att),
                           rmsnorm(conv_y, g_conv)], axis=-1)
    return cat @ w_out


def peer_ffn(hn, w_pq, sub_keys_a, sub_keys_b, expert_u, expert_v):
    n, s, d = hn.shape
    x = hn.reshape(n * s, d)
    tokens = n * s
    pad = (-tokens) % PEER_BLOCK
    xp = jnp.pad(x, ((0, pad), (0, 0))).reshape(-1, PEER_BLOCK, d)
    ka = sub_keys_a.astype(jnp.float32)
    kb = sub_keys_b.astype(jnp.float32)

    def block(xb):
        q = (xb @ w_pq).astype(jnp.float32).reshape(PEER_BLOCK, PEER_HEADS, PEER_QDIM)
        sa = jnp.einsum('thc,kc->thk', q[..., :PEER_HALF], ka)
        sb = jnp.einsum('thc,kc->thk', q[..., PEER_HALF:], kb)
        va, ia = lax.top_k(sa, PEER_TOPK)
        vb, ib = lax.top_k(sb, PEER_TOPK)
        cand = (va[..., :, None] + vb[..., None, :]).reshape(PEER_BLOCK, PEER_HEADS, -1)
        cid = (ia[..., :, None] * N_KEYS + ib[..., None, :]).reshape(PEER_BLOCK, PEER_HEADS, -1)
        top_s, top_i = lax.top_k(cand, PEER_TOPK)
        eid = jnp.take_along_axis(cid, top_i, axis=-1)
        gate = jax.nn.softmax(top_s, axis=-1)
        u = expert_u[eid]
        act = jax.nn.gelu(jnp.einsum('thkd,td->thk', u, xb).astype(jnp.float32), approximate=False)
        v = expert_v[eid]
        return jnp.einsum('thk,thkd->td', (gate * act).astype(xb.dtype), v)

    out = lax.map(block, xp).reshape(-1, d)[:tokens]
    return out.reshape(n, s, d)


def setup_inputs(seed: int = 0) -> dict:
    key = jax.random.key(seed)
    ks = jax.random.split(key, 24)
    f32 = jnp.float32
    nrm = lambda k, shape, sc: jax.random.normal(k, shape, f32) * sc
    gain = lambda k, shape: 1.0 + 0.02 * jax.random.normal(k, shape, f32)
    rows = [min(w, PAST_LEN) for (w, _) in DIL_PATTERNS]
    kv_shape = lambda r: (DEPTH, DEC_BATCH, r, ATT_SLOTS, HEAD_DIM)
    return {
        'x_prompt': nrm(ks[0], (BATCH, SEQ, D_MODEL), 1.0),
        'x_sample': nrm(ks[1], (DEC_BATCH, DEC_SEQ, D_MODEL), 1.0),
        'cache_k_g0': nrm(ks[2], kv_shape(rows[0]), 1.0),
        'cache_v_g0': nrm(ks[3], kv_shape(rows[0]), 1.0),
        'cache_k_g1': nrm(ks[4], kv_shape(rows[1]), 1.0),
        'cache_v_g1': nrm(ks[5], kv_shape(rows[1]), 1.0),
        'cache_k_g2': nrm(ks[6], kv_shape(rows[2]), 1.0),
        'cache_v_g2': nrm(ks[7], kv_shape(rows[2]), 1.0),
        'state_conv': nrm(ks[8], (DEPTH, DEC_BATCH, CONV_K - 1, CONV_WIDTH), 1.0),
        'norm_mix': gain(ks[9], (DEPTH, D_MODEL)),
        'w_in': nrm(ks[10], (DEPTH, D_MODEL, IN_WIDTH), D_MODEL ** -0.5),
        'conv_w': nrm(ks[11], (DEPTH, CONV_K, CONV_WIDTH), CONV_K ** -0.5),
        'norm_att_out': gain(ks[12], (DEPTH, ATT_WIDTH)),
        'norm_conv_out': gain(ks[13], (DEPTH, CONV_WIDTH)),
        'w_out': nrm(ks[14], (DEPTH, D_MODEL, D_MODEL), D_MODEL ** -0.5),
        'norm_ffn': gain(ks[15], (DEPTH, D_MODEL)),
        'w_pq': nrm(ks[16], (DEPTH, D_MODEL, PEER_HEADS * PEER_QDIM), D_MODEL ** -0.5),
        'sub_keys_a': nrm(ks[17], (DEPTH, N_KEYS, PEER_HALF), PEER_HALF ** -0.5),
        'sub_keys_b': nrm(ks[18], (DEPTH, N_KEYS, PEER_HALF), PEER_HALF ** -0.5),
        'expert_u': nrm(ks[19], (DEPTH, N_EXPERTS, D_MODEL), D_MODEL ** -0.5),
        'expert_v': nrm(ks[20], (DEPTH, N_EXPERTS, D_MODEL), PEER_HEADS ** -0.5),
        'norm_final': gain(ks[21], (D_MODEL,)),
    }


def reference(x_prompt, x_sample, cache_k_g0, cache_v_g0, cache_k_g1, cache_v_g1,
              cache_k_g2, cache_v_g2, state_conv, norm_mix, w_in, conv_w, norm_att_out,
              norm_conv_out, w_out, norm_ffn, w_pq, sub_keys_a, sub_keys_b, expert_u,
              expert_v, norm_final):
    slopes = alibi_slopes()
    caches_k = (cache_k_g0, cache_k_g1, cache_k_g2)
    caches_v = (cache_v_g0, cache_v_g1, cache_v_g2)
    s_len = x_prompt.shape[1]
    t_len = x_sample.shape[1]
    nk_p = [[] for _ in range(N_GROUPS)]
    nv_p = [[] for _ in range(N_GROUPS)]
    nk_s = [[] for _ in range(N_GROUPS)]
    nv_s = [[] for _ in range(N_GROUPS)]
    nc_p, nc_s = [], []
    hp, hs = x_prompt, x_sample
    for l in range(DEPTH):
        q, k, v, gate_b, u = split_projection(hp, norm_mix[l], w_in[l])
        outs, lses = [], []
        for g, (win, dil) in enumerate(DIL_PATTERNS):
            o, lse = dilated_prompt(q[:, :, g], k[:, :, g], v[:, :, g], win, dil, slopes[g])
            outs.append(o)
            lses.append(lse)
            rows = min(win, s_len)
            nk_p[g].append(k[:, s_len - rows:, g])
            nv_p[g].append(v[:, s_len - rows:, g])
        up = jnp.pad(u, ((0, 0), (CONV_K - 1, 0), (0, 0)))
        conv_y = gate_b * causal_conv(up, conv_w[l], s_len)
        nc_p.append(up[:, up.shape[1] - (CONV_K - 1):])
        hp = hp + mixer_output(combine_groups(outs, lses), conv_y,
                               norm_att_out[l], norm_conv_out[l], w_out[l])
        hp = hp + peer_ffn(rmsnorm(hp, norm_ffn[l]), w_pq[l], sub_keys_a[l], sub_keys_b[l],
                           expert_u[l], expert_v[l])

        q, k, v, gate_b, u = split_projection(hs, norm_mix[l], w_in[l])
        outs, lses = [], []
        for g, (win, dil) in enumerate(DIL_PATTERNS):
            k_all = jnp.concatenate([caches_k[g][l], k[:, :, g]], axis=1)
            v_all = jnp.concatenate([caches_v[g][l], v[:, :, g]], axis=1)
            o, lse = dilated_sample(q[:, :, g], k_all, v_all, win, dil, slopes[g],
                                    caches_k[g].shape[2])
            outs.append(o)
            lses.append(lse)
            rows = min(win, k_all.shape[1])
            nk_s[g].append(k_all[:, k_all.shape[1] - rows:])
            nv_s[g].append(v_all[:, v_all.shape[1] - rows:])
        up = jnp.concatenate([state_conv[l], u], axis=1)
        conv_y = gate_b * causal_conv(up, conv_w[l], t_len)
        nc_s.append(up[:, up.shape[1] - (CONV_K - 1):])
        hs = hs + mixer_output(combine_groups(outs, lses), conv_y,
                               norm_att_out[l], norm_conv_out[l], w_out[l])
        hs = hs + peer_ffn(rmsnorm(hs, norm_ffn[l]), w_pq[l], sub_keys_a[l], sub_keys_b[l],
                           expert_u[l], expert_v[l])
    y_prompt = rmsnorm(hp, norm_final)
    y_sample = rmsnorm(hs, norm_final)
    return (y_prompt, y_sample,
            jnp.stack(nk_p[0], 0), jnp.stack(nv_p[0], 0),
            jnp.stack(nk_p[1], 0), jnp.stack(nv_p[1], 0),
            jnp.stack(nk_p[2], 0), jnp.stack(nv_p[2], 0),
            jnp.stack(nc_p, 0),
            jnp.stack(nk_s[0], 0), jnp.stack(nv_s[0], 0),
            jnp.stack(nk_s[1], 0), jnp.stack(nv_s[1], 0),
            jnp.stack(nk_s[2], 0), jnp.stack(nv_s[2], 0),
            jnp.stack(nc_s, 0))
```

```python
import math
from contextlib import ExitStack
import numpy as np
import ml_dtypes
import concourse.bass as bass
import concourse.mybir as mybir
from concourse.bass_utils import run_bass_kernel_spmd

F32 = mybir.dt.float32
BF16 = mybir.dt.bfloat16
I32 = mybir.dt.int32
U32 = mybir.dt.uint32
AF = mybir.ActivationFunctionType
ALU = mybir.AluOpType
AX = mybir.AxisListType

D = 2048
NOWN = 1024
NCTX = 2048
NS = 32
NTOK = NCTX + NS
EPS = 1e-6
DILS = (1, 4, 16)
HALO = (128, 512, 1024)
CS = (NCTX - NOWN - 128, NCTX - NOWN - 512, 0)
CACHE_ROWS = (128, 512, 2048)
PHASES = ("A1", "A2", "A3", "A3s", "B")


class EngQ:
    def __init__(self, name, sem):
        self.name = name
        self.sem = sem
        self.cnt = 0
        self.ops = []
        self.seen = {}


class DSem:
    def __init__(self, sem):
        self.sem = sem
        self.cnt = 0


class Prog:
    def __init__(self, nc, sems, dsems):
        self.nc = nc
        self.q = {n: EngQ(n, sems[n]) for n in ("pe", "act", "dve", "pool", "sp")}
        self.dsems = [DSem(s) for s in dsems]
        self.nd = 0

    def new_dsem(self):
        d = self.dsems[self.nd]
        self.nd += 1
        return d

    def _waits(self, q, deps):
        waits = []
        for d in deps:
            if d is None:
                continue
            if isinstance(d, (list, tuple)) and (len(d) == 0 or not isinstance(d[0], (EngQ, DSem))):
                waits += self._waits(q, d)
                continue
            src, c = d
            if c <= 0:
                continue
            key = id(src)
            if q.seen.get(key, 0) >= c:
                continue
            q.seen[key] = c
            waits.append((src.sem, c))
        return waits

    def op(self, qn, fn, deps=(), signal=True):
        q = self.q[qn]
        waits = self._waits(q, deps)
        tok = None
        if signal:
            q.cnt += 1
            tok = (q, q.cnt)
        q.ops.append((waits, fn, q.sem if signal else None, 1))
        return tok

    def dma(self, qn, dsem, fn, deps=()):
        q = self.q[qn]
        waits = self._waits(q, deps)
        dsem.cnt += 16
        q.ops.append((waits, fn, dsem.sem, 16))
        return (dsem, dsem.cnt)

    def all_tokens(self):
        return [(q, q.cnt) for q in self.q.values() if q.name != "sp"] + [(d, d.cnt) for d in self.dsems]

    def barrier(self):
        toks = self.all_tokens()
        for q in self.q.values():
            waits = self._waits(q, toks)
            q.ops.append((waits, None, None, 0))

    def replay(self, block):
        def run(q):
            def f(eng):
                for waits, fn, sem, inc in q.ops:
                    for s, c in waits:
                        eng.wait_ge(s, c)
                    if fn is None:
                        continue
                    ins = fn(eng)
                    if sem is not None:
                        ins.then_inc(sem, inc)
            return f
        block.tensor(run(self.q["pe"]))
        block.scalar(run(self.q["act"]))
        block.vector(run(self.q["dve"]))
        block.gpsimd(run(self.q["pool"]))
        block.sync(run(self.q["sp"]))


class Arena:
    def __init__(self, nc, es, nbytes):
        self.t = es.enter_context(nc.sbuf_tensor("arena", [128, nbytes // 4], F32))
        self.off = 0
        self.cap = nbytes

    def alloc(self, free_shape, dt):
        n = 1
        for s in free_shape:
            n *= s
        esz = 4 if dt in (F32, I32, U32) else 2
        nb = (n * esz + 63) // 64 * 64
        assert self.off + nb <= self.cap, ("SBUF arena overflow", self.off, nb, self.cap)
        a = self.t[:, self.off // 4:(self.off + nb) // 4]
        self.off += nb
        if dt != F32:
            a = a.bitcast(dt)
        a = a[:, 0:n]
        if len(free_shape) == 2:
            a = a.rearrange("p (a b) -> p a b", b=free_shape[1])
        elif len(free_shape) == 3:
            a = a.rearrange("p (a b c) -> p a b c", b=free_shape[1], c=free_shape[2])
        return a


class Ring:
    def __init__(self, bufs, P=None):
        self.bufs = bufs
        self.free = [[] for _ in bufs]
        self.i = 0
        self.ds = [P.new_dsem() for _ in bufs] if P is not None else None

    def next(self):
        k = self.i % len(self.bufs)
        self.i += 1
        deps = self.free[k]
        self.free[k] = []
        return k, self.bufs[k], deps

    def release(self, k, toks):
        self.free[k] += [t for t in toks if t is not None]


def ss(start, n, step):
    return slice(start, start + (n - 1) * step + 1, step)


def build(phases=PHASES):
    nc = bass.Bass("TRN2", target_bir_lowering=False)

    def din(name, shape, dt=F32):
        return nc.dram_tensor(name, list(shape), dt, kind="ExternalInput").ap()

    def dout(name, shape, dt=F32):
        return nc.dram_tensor(name, list(shape), dt, kind="ExternalOutput").ap()

    xp = din("xp", [NCTX, D]); xs = din("xs", [NS, D])
    w_in = din("w_in", [D, 12288]); conv_w = din("conv_w", [3, 1024])
    g_mix = din("g_mix", [D]); g_att = din("g_att", [1024]); g_conv = din("g_conv", [1024])
    w_out = din("w_out", [D, D]); g_ffn = din("g_ffn", [D]); w_pq = din("w_pq", [D, D])
    ska = din("ska", [128, 128]); skb = din("skb", [128, 128])
    if "B" in phases:
        eu = din("eu", [16384, D]); ev = din("ev", [16384, D])
    g_fin = din("g_fin", [D])
    ck = [din("ck%d" % g, [4, CACHE_ROWS[g], 1024]) for g in range(3)]
    cv = [din("cv%d" % g, [4, CACHE_ROWS[g], 1024]) for g in range(3)]
    sconv = din("sconv", [4, 2, 1024])
    e0 = din("e0", [128, 16, 3, 128], BF16); e1 = din("e1", [128, 16, 3, 128], BF16)
    e2 = din("e2", [128, 16, 64], BF16)
    ident_b = din("ident_b", [128, 128], BF16); ident_f = din("ident_f", [128, 128])
    es_main = din("es_main", [128, 3, 8, 16]); es_tail = din("es_tail", [8, 3, 8, 16])
    selq = din("selq", [32, 32, 128], BF16); selo = din("selo", [128, 64], BF16)
    iota_d = din("iota256", [256])

    if "B" in phases:
        eu_bf = nc.dram_tensor("eu_bf", [16384, D], BF16, kind="Internal").ap()
        ev_bf = nc.dram_tensor("ev_bf", [16384, D], BF16, kind="Internal").ap()
        wo_bf = nc.dram_tensor("wo_bf", [D, D], BF16, kind="Internal").ap()
        wq_bf = nc.dram_tensor("wq_bf", [D, D], BF16, kind="Internal").ap()
    yp = dout("yp", [NOWN, D]); ys = dout("ys", [NS, D])
    okp = [dout("okp%d" % g, [HALO[g], 1024]) for g in range(3)]
    ovp = [dout("ovp%d" % g, [HALO[g], 1024]) for g in range(3)]
    oconvp = dout("oconvp", [2, 1024])
    oks = [dout("oks%d" % g, [4, CACHE_ROWS[g], 1024]) for g in range(3)]
    ovs = [dout("ovs%d" % g, [4, CACHE_ROWS[g], 1024]) for g in range(3)]
    oconvs = dout("oconvs", [4, 2, 1024])
    if 'dbg' in phases:
        dbg_ofin = dout("dbg_ofin", [8, NS, 130]); dbg_atts = dout("dbg_atts", [8, NS, 128], BF16)
        dbg_att = dout("dbg_att", [128, 8 * (NOWN + NS)], BF16); dbg_conv = dout("dbg_conv", [128, 8 * (NOWN + NS)], BF16)

    with ExitStack() as es:
        ar = Arena(nc, es, 190 * 1024)
        psb = [es.enter_context(nc.psum_tensor("ps%d" % i, [128, 512], F32)) for i in range(8)]
        sems = {n: es.enter_context(nc.semaphore("s_" + n)) for n in ("pe", "act", "dve", "pool", "sp")}
        dsems = [es.enter_context(nc.semaphore("d%d" % i)) for i in range(28)]
        P = Prog(nc, sems, dsems)
        ds_const = P.new_dsem(); ds_c2 = P.new_dsem(); ds_e = [P.new_dsem(), P.new_dsem()]
        ds_copy = P.new_dsem(); ds_c = P.new_dsem(); ds_cv = P.new_dsem()
        out_tokens = []

        ident = ar.alloc([128], BF16)
        identf = ar.alloc([128], F32)
        ones_b = ar.alloc([128], BF16)
        attT = ar.alloc([8, NOWN + NS], BF16)
        convT = ar.alloc([8, NOWN + NS], BF16)
        gatt = ar.alloc([8], F32); gconv = ar.alloc([8], F32)
        c_id = P.dma("sp", ds_const, lambda e: e.dma_start(out=ident, in_=ident_b))
        c_idf = P.dma("sp", ds_const, lambda e: e.dma_start(out=identf, in_=ident_f))
        c_ga = P.dma("sp", ds_const, lambda e: e.dma_start(out=gatt, in_=g_att.rearrange("(j c) -> c j", c=128), allow_slow_non_contiguous=True))
        c_gc = P.dma("sp", ds_const, lambda e: e.dma_start(out=gconv, in_=g_conv.rearrange("(j c) -> c j", c=128), allow_slow_non_contiguous=True))
        c_ones = P.op("pool", lambda e: e.memset(ones_b, 1.0))
        epsc = ar.alloc([1], F32)
        c_eps = P.op("pool", lambda e: e.memset(epsc, EPS))
        c_id = c_gc; c_idf = c_gc; c_ga = c_gc

        if "A3" in phases:
            for g in range(3):
                R = CACHE_ROWS[g]
                for b in range(4):
                    for src, dst in ((ck[g], oks[g]), (cv[g], ovs[g])):
                        out_tokens.append(P.dma("sp", ds_copy, lambda e, s=src, d=dst, b=b, R=R: e.dma_start(
                            out=d[b, 0:R - 8, :], in_=s[b, 8:R, :])))

        mark_persist = ar.off

        nd_persist = P.nd
        xnT = ar.alloc([16, NTOK], BF16)
        mark_xn = ar.off
        gmix_bc = ar.alloc([D], F32)
        xt_ring = Ring([ar.alloc([D], F32) for _ in range(2)], P)
        xnb_ring = Ring([ar.alloc([D], BF16) for _ in range(2)])
        junk_b = ar.alloc([D], BF16)
        stat = ar.alloc([64], F32)
        c_gm = P.dma("sp", ds_c2, lambda e: e.dma_start(out=gmix_bc, in_=g_mix.partition_broadcast(128)))
        psT_ring = Ring([psb[0][:].bitcast(BF16), psb[1][:].bitcast(BF16)])
        xn_ready = []
        for i in range(17):
            n = 128 if i < 16 else NS
            src = xp[i * 128:(i + 1) * 128, :] if i < 16 else xs
            k, xt, fdeps = xt_ring.next()
            ld = P.dma("sp", xt_ring.ds[k], lambda e, xt=xt, src=src, n=n: e.dma_start(out=xt[0:n], in_=src), deps=fdeps)
            sq = P.op("act", lambda e, xt=xt, n=n, i=i: e.activation(out=junk_b[0:n], in_=xt[0:n], func=AF.Square,
                                                                  accum_out=stat[0:n, i:i + 1]), deps=[ld])
            r1 = P.op("act", lambda e, n=n, i=i: e.activation(out=stat[0:n, 32 + i:33 + i], in_=stat[0:n, i:i + 1], func=AF.Sqrt,
                                                              scale=1.0 / D, bias=epsc[0:n, 0:1]), deps=[sq, c_eps])
            r2 = P.op("dve", lambda e, n=n, i=i: e.reciprocal(out=stat[0:n, 32 + i:33 + i], in_=stat[0:n, 32 + i:33 + i]), deps=[r1])
            kb, xnb, bdeps = xnb_ring.next()
            nm = P.op("dve", lambda e, xt=xt, xnb=xnb, n=n, i=i: e.scalar_tensor_tensor(
                out=xnb[0:n], in0=xt[0:n], scalar=stat[0:n, 32 + i:33 + i], in1=gmix_bc[0:n], op0=ALU.mult, op1=ALU.mult),
                deps=[r2, c_gm, bdeps])
            xt_ring.release(k, [nm, sq])
            tl = []
            for q4 in range(4):
                kp, pst, pdeps = psT_ring.next()
                for j in range(4):
                    kc = q4 * 4 + j
                    t = P.op("pe", lambda e, pst=pst, xnb=xnb, kc=kc, j=j, n=n: e.transpose(
                        out=pst[:, j * 128:j * 128 + n], in_=xnb[0:n, kc * 128:(kc + 1) * 128], identity=ident[0:n, 0:n]),
                        deps=[nm, c_id, pdeps], signal=(j == 3))
                c0 = i * 128
                cp = P.op("act", lambda e, pst=pst, q4=q4, c0=c0, n=n: e.activation(
                    out=xnT[:, q4 * 4:q4 * 4 + 4, c0:c0 + n],
                    in_=pst[:, 0:512].rearrange("p (a b) -> p a b", b=128)[:, :, 0:n], func=AF.Copy), deps=[t])
                psT_ring.release(kp, [cp])
                tl.append(cp)
            xnb_ring.release(kb, [t])
            xn_ready.append(tl)
        xn_all = [tl[-1] for tl in xn_ready]
        P.barrier()
        ar.off = mark_xn
        P.nd = nd_persist

        w_ring = Ring([ar.alloc([16, 128], BF16) for _ in range(3)], P)

        def load_w(col0):
            conv_step()
            k, wb, fdeps = w_ring.next()
            t = P.dma("pool", w_ring.ds[k], lambda e, wb=wb, col0=col0: e.dma_start(
                out=wb, in_=w_in[:, col0:col0 + 128].rearrange("(kc p) c -> p kc c", p=128)), deps=fdeps)
            return k, wb, t

        acc_ring = Ring([psb[0], psb[1]])
        conv_state = {"k": 0}
        mark_conv = ar.off
        ds_conv = [P.new_dsem(), P.new_dsem()]
        conv_tok = [None, None]

        def conv_step():
            k = conv_state["k"]
            if "B" not in phases or k >= 36:
                return
            conv_state["k"] = k + 1
            if k < 32:
                tab, tabb = (eu, eu_bf) if k < 16 else (ev, ev_bf)
                r0 = (k % 16) * 1024
            else:
                tab, tabb = (w_out, wo_bf) if k < 34 else (w_pq, wq_bf)
                r0 = (k % 2) * 1024
            conv_tok[k % 2] = P.dma("pool", ds_conv[k % 2], lambda e, tab=tab, tabb=tabb, r0=r0: e.dma_start(
                out=tabb[r0:r0 + 1024, :], in_=tab[r0:r0 + 1024, :]), deps=[conv_tok[k % 2]])

        mark_a = ar.off
        nd_a = P.nd

        if "A1" in phases:
            KT = [ar.alloc([NCTX - CS[g]], BF16) for g in range(3)]
            QT = [ar.alloc([NOWN], BF16) for g in range(3)]
            NVT = (9, 12, 16)
            VT = [ar.alloc([NVT[g], 128], BF16) for g in range(3)]
            Uacc = ar.alloc([NOWN], F32); Lacc = ar.alloc([NOWN], F32)
            vst_ring = Ring([ar.alloc([128], F32) for _ in range(2)], P)
            kst_ring = Ring([ar.alloc([128], F32) for _ in range(2)], P)
            e0p = [ar.alloc([2, 3, 128], BF16) for _ in range(2)]
            e1p = [ar.alloc([2, 3, 128], BF16) for _ in range(2)]
            e2p = [ar.alloc([2, 64], BF16) for _ in range(2)]
            pT_ring = [Ring([ar.alloc([256], BF16) for _ in range(2)]) for _ in range(2)]
            psS_ring = [Ring([psb[2 + 2 * h][:, 0:256], psb[3 + 2 * h][:, 0:256]]) for h in range(2)]
            psUL_ring = Ring([psb[6], psb[7]])
            e_free = [[], []]
            kq_free = []
            acc_free = []
            for p in range(8):
                es_ = p % 2
                ed = e_free[es_]; e_free[es_] = []
                te = [P.dma("sp", ds_e[es_], lambda e, p=p, s=es_: e.dma_start(out=e0p[s], in_=e0[:, 2 * p:2 * p + 2]), deps=ed),
                      P.dma("sp", ds_e[es_], lambda e, p=p, s=es_: e.dma_start(out=e1p[s], in_=e1[:, 2 * p:2 * p + 2]), deps=ed),
                      P.dma("sp", ds_e[es_], lambda e, p=p, s=es_: e.dma_start(out=e2p[s], in_=e2[:, 2 * p:2 * p + 2]), deps=ed)]
                te = te[-1]
                if 'noe' in phases:
                    pass
                kq_ready = [[], [], []]
                v_ready = [dict() for _ in range(3)]
                new_kq_free = []
                for g in range(3):
                    dil = DILS[g]
                    nctx = NCTX - CS[g]
                    k, wb, wt = load_w(3072 + g * 1024 + p * 128)
                    last = None
                    for t0 in range(0, nctx, 512):
                        n = min(512, nctx - t0)
                        ka, pacc, adeps = acc_ring.next()
                        for kc in range(16):
                            last = P.op("pe", lambda e, pacc=pacc, wb=wb, kc=kc, t0=t0, n=n, g=g: e.matmul(
                                pacc[:, 0:n], lhsT=wb[:, kc, :], rhs=xnT[:, kc, CS[g] + t0:CS[g] + t0 + n],
                                start=(kc == 0), stop=(kc == 15)), deps=[wt, adeps, xn_all], signal=(kc == 15))
                        cp = P.op("act", lambda e, pacc=pacc, t0=t0, n=n, g=g: e.activation(
                            out=KT[g][:, t0:t0 + n], in_=pacc[:, 0:n], func=AF.Copy), deps=[last, kq_free])
                        acc_ring.release(ka, [cp])
                        kq_ready[g].append(cp)
                    w_ring.release(k, [last])
                    nkt = HALO[g] // 128 if g < 2 else 8
                    for ti in range(nkt if 'nokout' not in phases else 0):
                        c0 = nctx - nkt * 128 + ti * 128
                        kk, pkb, kdeps = acc_ring.next()
                        pk = pkb[:].bitcast(BF16)[:, 0:128]
                        tr = P.op("pe", lambda e, pk=pk, c0=c0, g=g: e.transpose(out=pk, in_=KT[g][:, c0:c0 + 128], identity=ident),
                                  deps=[kq_ready[g][-1], kdeps])
                        ks, kst, sdeps = kst_ring.next()
                        cpk = P.op("dve", lambda e, pk=pk, kst=kst: e.tensor_copy(out=kst, in_=pk), deps=[tr, sdeps])
                        acc_ring.release(kk, [cpk])
                        do = P.dma("sp", kst_ring.ds[ks], lambda e, kst=kst, g=g, ti=ti, p=p: e.dma_start(
                            out=okp[g][ti * 128:(ti + 1) * 128, p * 128:(p + 1) * 128], in_=kst), deps=[cpk])
                        kst_ring.release(ks, [do])
                        out_tokens.append(do)
                        new_kq_free.append(tr)
                    k, wb, wt = load_w(g * 1024 + p * 128)
                    for t0 in range(0, NOWN, 512):
                        ka, pacc, adeps = acc_ring.next()
                        for kc in range(16):
                            last = P.op("pe", lambda e, pacc=pacc, wb=wb, kc=kc, t0=t0: e.matmul(
                                pacc[:, 0:512], lhsT=wb[:, kc, :], rhs=xnT[:, kc, NCTX - NOWN + t0:NCTX - NOWN + t0 + 512],
                                start=(kc == 0), stop=(kc == 15)), deps=[wt, adeps], signal=(kc == 15))
                        cp = P.op("act", lambda e, pacc=pacc, t0=t0, g=g: e.activation(
                            out=QT[g][:, t0:t0 + 512], in_=pacc[:, 0:512], func=AF.Copy), deps=[last, kq_free])
                        acc_ring.release(ka, [cp])
                        kq_ready[g].append(cp)
                    w_ring.release(k, [last])
                    k, wb, wt = load_w(6144 + g * 1024 + p * 128)
                    nblk = nctx // (128 * dil)
                    for r in range(dil if ('nov' not in phases and not ('vg0' in phases and g > 0) and not ('vg1' in phases and g != 1)) else 0):
                        for blk in range(nblk):
                            vi = r * nblk + blk
                            tstart = CS[g] + blk * 128 * dil + r
                            kv, pvb, vdeps = acc_ring.next()
                            pv = pvb[:, 0:128]
                            for kc in range(16):
                                last = P.op("pe", lambda e, pv=pv, wb=wb, kc=kc, tstart=tstart, dil=dil: e.matmul(
                                    pv, lhsT=xnT[:, kc, ss(tstart, 128, dil)], rhs=wb[:, kc, :],
                                    start=(kc == 0), stop=(kc == 15)), deps=[wt, vdeps], signal=(kc == 15))
                            if g == 0 and blk == nblk - 1:
                                rows = (0, 128, 1, 0)
                            elif g == 1 and blk == nblk - 1:
                                rows = (r, 512, 4, 0)
                            elif g == 2:
                                rows = (r, 1024, 16, 64)
                            else:
                                rows = None
                            if rows is not None and 'novout' not in phases:
                                r0, rn, rs, p0 = rows
                                ks, vst, sdeps = vst_ring.next()
                                cpo = P.op("act", lambda e, pv=pv, vst=vst: e.activation(out=vst, in_=pv, func=AF.Copy),
                                           deps=[last, sdeps])
                                cpv = P.op("dve", lambda e, vst=vst, g=g, vi=vi: e.tensor_copy(out=VT[g][:, vi, :], in_=vst),
                                           deps=[cpo, kq_free])
                                do = P.dma("sp", vst_ring.ds[ks], lambda e, vst=vst, g=g, p=p, r0=r0, rn=rn, rs=rs, p0=p0: e.dma_start(
                                    out=ovp[g][r0:rn:rs, p * 128:(p + 1) * 128], in_=vst[p0:128, :]), deps=[cpo])
                                vst_ring.release(ks, [do, cpv])
                                out_tokens.append(do)
                                rel = [cpo]
                            else:
                                cpv = P.op("dve", lambda e, pv=pv, g=g, vi=vi: e.tensor_copy(out=VT[g][:, vi, :], in_=pv),
                                           deps=[last, kq_free])
                                rel = [cpv]
                            v_ready[g][vi] = cpv
                            acc_ring.release(kv, rel)
                    w_ring.release(k, [last])

                units = []
                for n in range(8):
                    units.append(dict(g=0, nq=128, q=slice(n * 128, n * 128 + 128), o=slice(n * 128, n * 128 + 128),
                                      kts=[(slice(n * 128, n * 128 + 128), n, e0p[es_][:, :, 0 if n == 0 else 1, :]),
                                           (slice((n + 1) * 128, (n + 2) * 128), n + 1, e0p[es_][:, :, 2, :])], first=True))
                for r in range(4):
                    for n in (1, 2):
                        qs = ss((n - 1) * 512 + r, 128, 4)
                        units.append(dict(g=1, nq=128, q=qs, o=qs,
                                          kts=[(ss((n - 1) * 512 + r, 128, 4), r * 3 + n - 1,
                                                e1p[es_][:, :, 0 if n == 1 else 1, :]),
                                               (ss(n * 512 + r, 128, 4), r * 3 + n, e1p[es_][:, :, 2, :])],
                                          first=False))
                for r in range(16):
                    qs = slice(r, 1024, 16)
                    units.append(dict(g=2, nq=64, q=qs, o=qs, kts=[(slice(r, 2048, 16), r, e2p[es_])], first=False))
                last_mask = None
                if 'noattn' in phases:
                    units = []
                    a1 = a2 = None
                def stage_s(u):
                    g = u["g"]; nq = u["nq"]; nk = len(u["kts"])
                    pts = []
                    mk = None
                    for h in range(2):
                        hs = slice(h * 64, h * 64 + 64)
                        ksl, psS, sdeps = psS_ring[h].next()
                        for ki, (kcols, vi, etab) in enumerate(u["kts"]):
                            mm = P.op("pe", lambda e, psS=psS, g=g, hs=hs, kcols=kcols, q=u["q"], ki=ki, nq=nq: e.matmul(
                                psS[:, ki * 128:ki * 128 + nq], lhsT=KT[g][hs, kcols], rhs=QT[g][hs, q], start=True, stop=True),
                                deps=[kq_ready[g], sdeps], signal=(ki == nk - 1))
                        kp, pT, pdeps = pT_ring[h].next()
                        ex = P.op("act", lambda e, psS=psS, pT=pT, nk=nk, nq=nq: e.activation(
                            out=pT[:, 0:nk * 128].rearrange("p (a b) -> p a b", b=128)[:, :, 0:nq],
                            in_=psS[:, 0:nk * 128].rearrange("p (a b) -> p a b", b=128)[:, :, 0:nq], func=AF.Exp, scale=0.125),
                            deps=[mm, pdeps])
                        psS_ring[h].release(ksl, [ex])
                        for ki, (kcols, vi, etab) in enumerate(u["kts"]):
                            mk = P.op("pool", lambda e, pT=pT, ki=ki, nq=nq, etab=etab, h=h: e.tensor_tensor(
                                out=pT[:, ki * 128:ki * 128 + nq], in0=pT[:, ki * 128:ki * 128 + nq], in1=etab[:, h, 0:nq], op=ALU.mult),
                                deps=[ex, te])
                        pts.append((kp, pT, mk))
                    u["pts"] = pts
                    return mk

                def stage_pv(u):
                    g = u["g"]; nq = u["nq"]; nk = len(u["kts"])
                    ku, pUL, udeps = psUL_ring.next()
                    pU = pUL[:, 0:128]; pL = pUL[:, 128:256]
                    ml = None
                    for h in range(2):
                        hs = slice(h * 64, h * 64 + 64)
                        kp, pT, mk = u["pts"][h]
                        for ki, (kcols, vi, etab) in enumerate(u["kts"]):
                            P.op("pe", lambda e, pU=pU, hs=hs, g=g, vi=vi, pT=pT, ki=ki, nq=nq, nk=nk: e.matmul(
                                pU[hs, 0:nq], lhsT=VT[g][:, vi, hs], rhs=pT[:, ki * 128:ki * 128 + nq], start=(ki == 0), stop=(ki == nk - 1)),
                                deps=[mk, v_ready[g][vi], udeps], signal=False)
                        for ki, (kcols, vi, etab) in enumerate(u["kts"]):
                            ml = P.op("pe", lambda e, pL=pL, hs=hs, pT=pT, ki=ki, nq=nq, nk=nk: e.matmul(
                                pL[hs, 0:nq], lhsT=ones_b[:, 0:64], rhs=pT[:, ki * 128:ki * 128 + nq], start=(ki == 0), stop=(ki == nk - 1)),
                                deps=[c_ones], signal=(ki == nk - 1))
                        pT_ring[h].release(kp, [ml])
                    if u["first"]:
                        a1 = P.op("dve", lambda e, pU=pU, o=u["o"], nq=nq: e.tensor_copy(out=Uacc[:, o], in_=pU[:, 0:nq]), deps=[ml, acc_free])
                        a2 = P.op("dve", lambda e, pL=pL, o=u["o"], nq=nq: e.tensor_copy(out=Lacc[:, o], in_=pL[:, 0:nq]), deps=[ml, acc_free])
                    else:
                        a1 = P.op("dve", lambda e, pU=pU, o=u["o"], nq=nq: e.tensor_tensor(out=Uacc[:, o], in0=Uacc[:, o], in1=pU[:, 0:nq], op=ALU.add), deps=[ml])
                        a2 = P.op("dve", lambda e, pL=pL, o=u["o"], nq=nq: e.tensor_tensor(out=Lacc[:, o], in0=Lacc[:, o], in1=pL[:, 0:nq], op=ALU.add), deps=[ml])
                    psUL_ring.release(ku, [a1, a2])
                    new_kq_free.append(ml)
                    return a1, a2

                for ui in range(len(units) + 1):
                    if ui < len(units):
                        last_mask = stage_s(units[ui])
                    if ui >= 1:
                        a1, a2 = stage_pv(units[ui - 1])
                e_free[es_] = [last_mask]
                if 'nofin' in phases:
                    continue
                f1 = P.op("dve", lambda e: e.reciprocal(out=Lacc, in_=Lacc), deps=[a1, a2])
                f2 = P.op("dve", lambda e, p=p: e.tensor_tensor(out=attT[:, p, 0:NOWN], in0=Uacc, in1=Lacc, op=ALU.mult), deps=[f1])
                acc_free = [f2]
                kq_free = new_kq_free
            P.barrier()
        ar.off = mark_a
        P.nd = nd_a

        if "A2" in phases:
            NL = NOWN + 2 + NS
            C0 = NCTX - NOWN - 2
            gcS = ar.alloc([NL], F32); gbS = ar.alloc([NL], F32); uT = ar.alloc([NL], F32)
            cy = ar.alloc([NOWN], F32); cys = ar.alloc([4, 8], F32)
            ucat = ar.alloc([4, 10], F32)
            wc = ar.alloc([8, 3], F32)
            for kk_ in range(3):
                c_wc = P.dma("sp", ds_c2, lambda e, kk_=kk_: e.dma_start(out=wc[:, :, kk_], in_=conv_w[kk_].rearrange("(j c) -> c j", c=128), allow_slow_non_contiguous=True))
            blocks = [(0, 512), (512, 512), (1024, NL - 1024)]
            cacc = [Ring([psb[2 * t], psb[2 * t + 1]]) for t in range(3)]
            prev = []
            for j in range(8):
                tiles = {}
                for wi, col in enumerate((9216, 10240, 11264)):
                    k, wb, wt = load_w(col + j * 128)
                    for bi, (t0, n) in enumerate(blocks):
                        ka, pacc, adeps = cacc[bi].next()
                        for kc in range(16):
                            last = P.op("pe", lambda e, pacc=pacc, wb=wb, kc=kc, t0=t0, n=n: e.matmul(
                                pacc[:, 0:n], lhsT=wb[:, kc, :], rhs=xnT[:, kc, C0 + t0:C0 + t0 + n], start=(kc == 0), stop=(kc == 15)),
                                deps=[wt, adeps, xn_all], signal=(kc == 15))
                        if wi == 0:
                            cp = P.op("act", lambda e, pacc=pacc, t0=t0, n=n: e.activation(out=gbS[:, t0:t0 + n], in_=pacc[:, 0:n], func=AF.Copy), deps=[last, prev])
                        elif wi == 1:
                            cp = P.op("act", lambda e, pacc=pacc, t0=t0, n=n: e.activation(out=gcS[:, t0:t0 + n], in_=pacc[:, 0:n], func=AF.Copy), deps=[last, prev])
                        else:
                            cp = P.op("dve", lambda e, pacc=pacc, t0=t0, n=n: e.tensor_tensor(out=uT[:, t0:t0 + n], in0=pacc[:, 0:n], in1=gcS[:, t0:t0 + n], op=ALU.mult),
                                      deps=[last, tiles[(1, bi)], prev])
                        tiles[(wi, bi)] = cp
                        cacc[bi].release(ka, [cp])
                    w_ring.release(k, [last])
                rdy = list(tiles.values())
                o1 = P.op("dve", lambda e, j=j: e.tensor_scalar(out=cy, in0=uT[:, 0:NOWN], scalar1=wc[:, j, 0:1], scalar2=None, op0=ALU.mult), deps=[rdy, c_wc])
                o2 = P.op("dve", lambda e, j=j: e.scalar_tensor_tensor(out=cy, in0=uT[:, 1:NOWN + 1], scalar=wc[:, j, 1:2], in1=cy, op0=ALU.mult, op1=ALU.add), deps=[o1])
                o3 = P.op("dve", lambda e, j=j: e.scalar_tensor_tensor(out=cy, in0=uT[:, 2:NOWN + 2], scalar=wc[:, j, 2:3], in1=cy, op0=ALU.mult, op1=ALU.add), deps=[o2])
                o4 = P.op("dve", lambda e, j=j: e.tensor_tensor(out=convT[:, j, 0:NOWN], in0=cy, in1=gbS[:, 2:NOWN + 2], op=ALU.mult), deps=[o3])
                for tt in range(2):
                    ls = P.dma("sp", ds_c, lambda e, j=j, tt=tt: e.dma_start(out=ucat[:, :, tt], in_=sconv[:, tt, j * 128:(j + 1) * 128].rearrange("b c -> c b"), allow_slow_non_contiguous=True), deps=[prev])
                s0 = P.op("dve", lambda e: e.tensor_copy(out=ucat[:, :, 2:10], in_=uT[:, NOWN + 2:NL].rearrange("p (b t) -> p b t", t=8)), deps=[rdy, prev])
                s1 = P.op("dve", lambda e, j=j: e.tensor_scalar(out=cys, in0=ucat[:, :, 0:8], scalar1=wc[:, j, 0:1], scalar2=None, op0=ALU.mult), deps=[s0, ls])
                s2 = P.op("dve", lambda e, j=j: e.scalar_tensor_tensor(out=cys, in0=ucat[:, :, 1:9], scalar=wc[:, j, 1:2], in1=cys, op0=ALU.mult, op1=ALU.add), deps=[s1])
                s3 = P.op("dve", lambda e, j=j: e.scalar_tensor_tensor(out=cys, in0=ucat[:, :, 2:10], scalar=wc[:, j, 2:3], in1=cys, op0=ALU.mult, op1=ALU.add), deps=[s2])
                s4 = P.op("dve", lambda e, j=j: e.tensor_tensor(out=convT[:, j, NOWN:NOWN + NS].rearrange("p (b t) -> p b t", t=8), in0=cys,
                                                               in1=gbS[:, NOWN + 2:NL].rearrange("p (b t) -> p b t", t=8), op=ALU.mult), deps=[s3])
                d1 = P.dma("sp", ds_cv, lambda e, j=j: e.dma_start(out=oconvp[:, j * 128:(j + 1) * 128].rearrange("t c -> c t"), in_=uT[:, NOWN:NOWN + 2], allow_slow_non_contiguous=True), deps=[rdy])
                for tt in range(2):
                    d2 = P.dma("sp", ds_cv, lambda e, j=j, tt=tt: e.dma_start(out=oconvs[:, tt, j * 128:(j + 1) * 128].rearrange("b c -> c b"), in_=ucat[:, :, 8 + tt], allow_slow_non_contiguous=True), deps=[s0])
                out_tokens += [d1, d2]
                prev = [o4, s4, d2, o3, s3]
            P.barrier()
        ar.off = mark_a
        P.nd = nd_a

        if "A3s" in phases:
            ar.off = mark_conv
            selq_sb = ar.alloc([32, 128], BF16); selo_sb = ar.alloc([64], BF16)
            esm = ar.alloc([3, 8, 16], F32); est = ar.alloc([3, 8, 16], F32)
            P.dma("sp", ds_c2, lambda e: e.dma_start(out=selq_sb[0:32], in_=selq))
            P.dma("sp", ds_c2, lambda e: e.dma_start(out=selo_sb, in_=selo))
            P.dma("sp", ds_c2, lambda e: e.dma_start(out=esm, in_=es_main))
            c3_all = P.dma("sp", ds_c2, lambda e: e.dma_start(out=est[0:8], in_=es_tail))
            qkv_s = [ar.alloc([3, 128], F32) for _ in range(3)]
            qb = [ar.alloc([128], BF16) for _ in range(3)]
            Qb_ring = Ring([ar.alloc([8, 128], F32) for _ in range(2)])
            kc_ring = Ring([ar.alloc([8, 128], F32) for _ in range(2)], P)
            vc_ring = Ring([ar.alloc([8, 128], F32) for _ in range(2)], P)
            prod = ar.alloc([8, 128], F32); S_ = ar.alloc([16], F32); Pm = ar.alloc([16], F32)
            WP_ring = Ring([ar.alloc([8, 130], BF16) for _ in range(2)])
            kn_ring = Ring([ar.alloc([128], F32) for _ in range(2)], P)
            vn_ring = Ring([ar.alloc([128], F32) for _ in range(2)], P)
            prod_t = ar.alloc([8, 128], F32); St = ar.alloc([16], F32); Pt = ar.alloc([16], F32)
            WPt_ring = Ring([ar.alloc([8, 130], BF16) for _ in range(2)])
            ofin = ar.alloc([130], F32); rl_ = ar.alloc([2], F32); atts = ar.alloc([128], BF16)
            ds_o3 = [P.new_dsem() for _ in range(3)]
            qsets = Ring([(psb[2], psb[3]), (psb[4], psb[5])])
            obank_free = [[], []]
            qkv_free = [[], [], []]
            th = lambda ap: ap.rearrange("p (t h) -> p t h", h=2)
            hd = lambda ap: ap.rearrange("p t (h d) -> p t h d", d=64)
            for p in range(8):
                out_acc = psb[6 + p % 2][0:NS, 0:130]
                first_mm = [True]
                evs = []
                new_qkv_free = [[], [], []]
                qbcs = []
                for g in range(3):
                    R = CACHE_ROWS[g]
                    ka, pacc, adeps = acc_ring.next()
                    for wi, col in enumerate((g * 1024 + p * 128, 3072 + g * 1024 + p * 128, 6144 + g * 1024 + p * 128)):
                        k, wb, wt = load_w(col)
                        for kc in range(16):
                            last = P.op("pe", lambda e, pacc=pacc, wi=wi, kc=kc, wb=wb: e.matmul(
                                pacc[0:NS, wi * 128:(wi + 1) * 128], lhsT=xnT[:, kc, NCTX:NCTX + NS], rhs=wb[:, kc, :], start=(kc == 0), stop=(kc == 15)),
                                deps=[wt, adeps, xn_all], signal=(kc == 15))
                        w_ring.release(k, [last])
                    ev_ = P.op("act", lambda e, pacc=pacc, g=g: e.activation(out=qkv_s[g][0:NS], in_=pacc[0:NS, 0:384].rearrange("p (a b) -> p a b", b=128), func=AF.Copy),
                               deps=[last, qkv_free[g]])
                    acc_ring.release(ka, [ev_])
                    evs.append(ev_)
                    qbc = P.op("pool", lambda e, g=g: e.tensor_copy(out=qb[g][0:NS], in_=qkv_s[g][0:NS, 0, :]), deps=[ev_, qkv_free[g]])
                    qbcs.append(qbc)
                    for b in range(4):
                        for wi, dst in ((1, oks[g]), (2, ovs[g])):
                            do = P.dma("sp", ds_o3[g], lambda e, dst=dst, b=b, R=R, p=p, g=g, wi=wi: e.dma_start(
                                out=dst[b, R - 8:R, p * 128:(p + 1) * 128], in_=qkv_s[g][8 * b:8 * b + 8, wi, :]), deps=[ev_])
                    new_qkv_free[g].append(do)
                mo = None
                for g in range(3):
                    dil = DILS[g]
                    nr = min(dil, 8)
                    for b in range(4):
                        ks, (pq0, pq1), qdeps = qsets.next()
                        mq3 = mq7 = None
                        for t in range(8):
                            bank = pq0 if t < 4 else pq1
                            tok = P.op("pe", lambda e, bank=bank, t=t, b=b, g=g: e.matmul(
                                bank[:, (t % 4) * 128:(t % 4 + 1) * 128], lhsT=selq_sb[0:NS, 8 * b + t, :], rhs=qb[g][0:NS, :], start=True, stop=True),
                                deps=[qbcs[g], qdeps, c3_all], signal=(t in (3, 7)))
                            if t == 3:
                                mq3 = tok
                            if t == 7:
                                mq7 = tok
                        kq, Qb, qbdeps = Qb_ring.next()
                        c0_ = P.op("dve", lambda e, Qb=Qb, pq0=pq0: e.tensor_copy(out=Qb[:, 0:4, :], in_=pq0[:, :].rearrange("p (a b) -> p a b", b=128)), deps=[mq3, qbdeps])
                        c1_ = P.op("act", lambda e, Qb=Qb, pq1=pq1: e.activation(out=Qb[:, 4:8, :], in_=pq1[:, :].rearrange("p (a b) -> p a b", b=128), func=AF.Copy), deps=[mq7, qbdeps])
                        qsets.release(ks, [c0_, c1_])
                        kk, Kc, kdeps = kc_ring.next()
                        lk = P.dma("sp", kc_ring.ds[kk], lambda e, Kc=Kc, g=g, b=b, p=p, dil=dil, nr=nr: e.dma_start(
                            out=Kc[:, 0:nr, :], in_=ck[g][b, :, p * 128:(p + 1) * 128].rearrange("(m r) c -> m r c", r=dil)[:, 0:nr, :]), deps=[kdeps])
                        kv_, Vc, vdeps = vc_ring.next()
                        lv = P.dma("sp", vc_ring.ds[kv_], lambda e, Vc=Vc, g=g, b=b, p=p, dil=dil, nr=nr: e.dma_start(
                            out=Vc[:, 0:nr, :], in_=cv[g][b, :, p * 128:(p + 1) * 128].rearrange("(m r) c -> m r c", r=dil)[:, 0:nr, :]), deps=[vdeps])
                        if g == 0:
                            p1 = P.op("dve", lambda e, Qb=Qb, Kc=Kc: e.tensor_tensor(out=prod, in0=Qb, in1=Kc[:, 0:1, :].broadcast_to([128, 8, 128]), op=ALU.mult), deps=[c0_, c1_, lk])
                        elif g == 2:
                            p1 = P.op("dve", lambda e, Qb=Qb, Kc=Kc: e.tensor_tensor(out=prod, in0=Qb, in1=Kc[:, 0:8, :], op=ALU.mult), deps=[c0_, c1_, lk])
                        else:
                            P.op("dve", lambda e, Qb=Qb, Kc=Kc: e.tensor_tensor(out=prod[:, 0:4, :], in0=Qb[:, 0:4, :], in1=Kc[:, 0:4, :], op=ALU.mult), deps=[c0_, c1_, lk])
                            p1 = P.op("dve", lambda e, Qb=Qb, Kc=Kc: e.tensor_tensor(out=prod[:, 4:8, :], in0=Qb[:, 4:8, :], in1=Kc[:, 0:4, :], op=ALU.mult), deps=[c0_, c1_, lk])
                        kc_ring.release(kk, [p1])
                        r_ = P.op("dve", lambda e: e.tensor_reduce(out=S_, in_=prod.rearrange("p t (h d) -> p (t h) d", d=64), axis=AX.X, op=ALU.add), deps=[p1])
                        ex = P.op("act", lambda e: e.activation(out=Pm, in_=S_, func=AF.Exp, scale=0.125), deps=[r_])
                        mk = P.op("dve", lambda e, g=g, p=p: e.tensor_tensor(out=th(Pm), in0=th(Pm), in1=esm[:, g, :, 2 * p:2 * p + 2], op=ALU.mult), deps=[ex, c3_all])
                        kw, WP, wdeps = WP_ring.next()
                        pm4 = lambda lo, hi: th(Pm)[:, lo:hi, :].unsqueeze(3).broadcast_to([128, hi - lo, 2, 64])
                        if g == 0:
                            w1 = P.op("dve", lambda e, WP=WP, Vc=Vc: e.tensor_tensor(out=hd(WP[:, :, 0:128]), in0=hd(Vc[:, 0:1, :].broadcast_to([128, 8, 128])), in1=pm4(0, 8), op=ALU.mult), deps=[mk, lv, wdeps])
                        elif g == 2:
                            w1 = P.op("dve", lambda e, WP=WP, Vc=Vc: e.tensor_tensor(out=hd(WP[:, :, 0:128]), in0=hd(Vc[:, 0:8, :]), in1=pm4(0, 8), op=ALU.mult), deps=[mk, lv, wdeps])
                        else:
                            P.op("dve", lambda e, WP=WP, Vc=Vc: e.tensor_tensor(out=hd(WP[:, 0:4, 0:128]), in0=hd(Vc[:, 0:4, :]), in1=pm4(0, 4), op=ALU.mult), deps=[mk, lv, wdeps])
                            w1 = P.op("dve", lambda e, WP=WP, Vc=Vc: e.tensor_tensor(out=hd(WP[:, 4:8, 0:128]), in0=hd(Vc[:, 0:4, :]), in1=pm4(4, 8), op=ALU.mult), deps=[mk, lv, wdeps])
                        w2 = P.op("dve", lambda e, WP=WP: e.tensor_copy(out=WP[:, :, 128:130], in_=th(Pm)), deps=[w1])
                        vc_ring.release(kv_, [w1])
                        for t in range(8):
                            j = 8 * b + t
                            mo = P.op("pe", lambda e, WP=WP, t=t, j=j, st=first_mm[0], out_acc=out_acc: e.matmul(out_acc, lhsT=selo_sb[:, 31 - j:63 - j], rhs=WP[:, t, :], start=st, stop=False),
                                      deps=[w2, obank_free[p % 2], c3_all], signal=(t == 7))
                            first_mm[0] = False
                        WP_ring.release(kw, [mo])
                        kn, knew, kndeps = kn_ring.next()
                        lkn = P.dma("sp", kn_ring.ds[kn], lambda e, knew=knew, g=g, b=b: e.dma_start(out=knew[0:8], in_=qkv_s[g][8 * b:8 * b + 8, 1, :]), deps=[evs[g], kndeps])
                        vn, vnew, vndeps = vn_ring.next()
                        lvn = P.dma("sp", vn_ring.ds[vn], lambda e, vnew=vnew, g=g, b=b: e.dma_start(out=vnew[0:8], in_=qkv_s[g][8 * b:8 * b + 8, 2, :]), deps=[evs[g], vndeps])
                        new_qkv_free[g] += [lkn, lvn]
                        pt1 = P.op("dve", lambda e, Qb=Qb, knew=knew: e.tensor_tensor(out=prod_t[0:8], in0=Qb[0:8], in1=knew[0:8].unsqueeze(1).broadcast_to([8, 8, 128]), op=ALU.mult), deps=[c0_, c1_, lkn])
                        Qb_ring.release(kq, [p1, pt1])
                        kn_ring.release(kn, [pt1])
                        rt = P.op("dve", lambda e: e.tensor_reduce(out=St[0:8], in_=prod_t[0:8].rearrange("p t (h d) -> p (t h) d", d=64), axis=AX.X, op=ALU.add), deps=[pt1])
                        ext = P.op("act", lambda e: e.activation(out=Pt[0:8], in_=St[0:8], func=AF.Exp, scale=0.125), deps=[rt])
                        mkt = P.op("dve", lambda e, g=g, p=p: e.tensor_tensor(out=th(Pt[0:8]), in0=th(Pt[0:8]), in1=est[0:8, g, :, 2 * p:2 * p + 2], op=ALU.mult), deps=[ext, c3_all])
                        kwt, WPt, wtdeps = WPt_ring.next()
                        wt1 = P.op("dve", lambda e, WPt=WPt, vnew=vnew: e.tensor_tensor(
                            out=hd(WPt[0:8, :, 0:128]), in0=vnew[0:8].rearrange("p (h d) -> p h d", d=64).unsqueeze(1).broadcast_to([8, 8, 2, 64]),
                            in1=th(Pt[0:8]).unsqueeze(3).broadcast_to([8, 8, 2, 64]), op=ALU.mult), deps=[mkt, lvn, wtdeps])
                        wt2 = P.op("dve", lambda e, WPt=WPt: e.tensor_copy(out=WPt[0:8, :, 128:130], in_=th(Pt[0:8])), deps=[wt1])
                        vn_ring.release(vn, [wt1])
                        lastu = (g == 2 and b == 3)
                        for t in range(8):
                            j = 8 * b + t
                            mo = P.op("pe", lambda e, WPt=WPt, t=t, j=j, sp_=(lastu and t == 7), out_acc=out_acc: e.matmul(out_acc, lhsT=selo_sb[0:8, 31 - j:63 - j], rhs=WPt[0:8, t, :], start=False, stop=sp_),
                                      deps=[wt2], signal=(t == 7))
                        WPt_ring.release(kwt, [mo])
                f1_ = P.op("dve", lambda e, out_acc=out_acc: e.tensor_copy(out=ofin[0:NS], in_=out_acc), deps=[mo])
                obank_free[p % 2] = [f1_]
                f2_ = P.op("dve", lambda e: e.reciprocal(out=rl_[0:NS], in_=ofin[0:NS, 128:130]), deps=[f1_])
                f3_ = P.op("dve", lambda e: e.tensor_tensor(out=atts[0:NS].rearrange("p (h d) -> p h d", d=64), in0=ofin[0:NS, 0:128].rearrange("p (h d) -> p h d", d=64),
                                                           in1=rl_[0:NS].unsqueeze(2).broadcast_to([NS, 2, 64]), op=ALU.mult), deps=[f2_])
                if 'dbg' in phases:
                    dd1 = P.dma("sp", ds_copy, lambda e, p=p: e.dma_start(out=dbg_ofin[p], in_=ofin[0:NS]), deps=[f3_])
                    dd2 = P.dma("sp", ds_copy, lambda e, p=p: e.dma_start(out=dbg_atts[p], in_=atts[0:NS]), deps=[f3_])
                    obank_free[p % 2] = [f1_, dd1, dd2]
                ka, pacc, adeps = acc_ring.next()
                pab = pacc[:].bitcast(BF16)[:, 0:NS]
                tr_ = P.op("pe", lambda e, pab=pab: e.transpose(out=pab, in_=atts[0:NS, :], identity=ident[0:NS, 0:NS]), deps=[f3_, adeps, c_id])
                cpa = P.op("act", lambda e, pab=pab, p=p: e.activation(out=attT[:, p, NOWN:NOWN + NS], in_=pab, func=AF.Copy), deps=[tr_])
                acc_ring.release(ka, [cpa])
                qkv_free = new_qkv_free
            P.barrier()

        if "B" in phases:
            ar.off = mark_persist
            P.nd = nd_persist
            gffn_bc = ar.alloc([D], F32); gfin_bc = ar.alloc([D], F32)
            kaT = ar.alloc([128], F32); kbT = ar.alloc([128], F32); sk_st = ar.alloc([128], F32)
            WB = 256
            wring = Ring([ar.alloc([16, WB], BF16) for _ in range(2)], P)
            gring = Ring([ar.alloc([D], BF16) for _ in range(9)], P)
            xt = ar.alloc([D], F32); xn2b = ar.alloc([D], BF16)
            h1s = [ar.alloc([D], F32)] * 2; xn2s = [ar.alloc([D], F32) for _ in range(2)]
            eids = [ar.alloc([128], I32) for _ in range(2)]; gates = [ar.alloc([8, 16], F32) for _ in range(2)]
            accb = ar.alloc([D], F32)
            h1_free = [None]
            attg = ar.alloc([16, 128], BF16); xn2T = ar.alloc([16, 128], BF16)
            sqb = xn2T
            qT = ar.alloc([16, 128], F32); sc = ar.alloc([16, 128], F32); scw = ar.alloc([128], F32)
            v16 = ar.alloc([16, 16], F32); i16 = ar.alloc([16, 16], U32); idxf = ar.alloc([16, 16], F32)
            cand = ar.alloc([256], F32); cid = ar.alloc([256], F32); cw = ar.alloc([256], F32); junk256 = ar.alloc([256], F32)
            iota_f = ar.alloc([256], F32); pos_u = ar.alloc([8, 16], U32); posf = ar.alloc([8, 16], F32)
            ts = ar.alloc([8, 16], F32); tsub = ar.alloc([8, 16], F32); ge = ar.alloc([8, 16], F32); zs = ar.alloc([8], F32)
            eidf = ar.alloc([128], F32)
            dots = ar.alloc([128], F32); actv = ar.alloc([128], F32); coef = ar.alloc([128], F32); st2 = ar.alloc([16], F32); st3 = ar.alloc([8], F32)
            ds_b = P.new_dsem(); ds_y = P.new_dsem()
            cb1 = P.dma("sp", ds_b, lambda e: e.dma_start(out=gffn_bc, in_=g_ffn.partition_broadcast(128)))
            P.dma("sp", ds_b, lambda e: e.dma_start(out=iota_f, in_=iota_d.partition_broadcast(128)))
            cb2 = P.dma("sp", ds_b, lambda e: e.dma_start(out=gfin_bc, in_=g_fin.partition_broadcast(128)))
            prevt = None
            for src, dst in ((ska, kaT), (skb, kbT)):
                l0 = P.dma("sp", ds_b, lambda e, src=src: e.dma_start(out=sk_st, in_=src), deps=[prevt])
                t0_ = P.op("pe", lambda e: e.transpose(out=psb[0][:, 0:128], in_=sk_st, identity=identf), deps=[l0, c_idf, prevt])
                prevt = P.op("act", lambda e, dst=dst: e.activation(out=dst, in_=psb[0][:, 0:128], func=AF.Copy), deps=[t0_])
            P.barrier()

            def load_wb(wsrc, cb):
                k, wb, fdeps = wring.next()
                t = P.dma("sp", wring.ds[k], lambda e, wb=wb, wsrc=wsrc, cb=cb: e.dma_start(
                    out=wb, in_=wsrc[:, cb * WB:(cb + 1) * WB].rearrange("(kc p) c -> p kc c", p=128)), deps=fdeps)
                return k, wb, t

            def rstd_of(ssq_ap, out_ap, dim, deps):
                a = P.op("act", lambda e: e.activation(out=out_ap, in_=ssq_ap, func=AF.Sqrt, scale=1.0 / dim, bias=epsc[0:out_ap.shape[0], 0:1]), deps=deps + [c_eps])
                return P.op("dve", lambda e: e.reciprocal(out=out_ap, in_=out_ap), deps=[a])

            ntiles = (9 if 'A3s' in phases else 8) if 'b1' not in phases else 1

            def front(i):
                n = 128 if i < 8 else NS
                c0 = i * 128
                h1 = h1s[i % 2]; xn2 = xn2s[i % 2]; eid = eids[i % 2]; gate = gates[i % 2]
                src = xp[NCTX - NOWN + c0:NCTX - NOWN + c0 + 128, :] if i < 8 else xs
                lx = P.dma("sp", ds_b, lambda e: e.dma_start(out=xt[0:n], in_=src))
                g1_ = P.op("pool", lambda e: e.tensor_tensor(out=attg[:, 0:8, 0:n], in0=attT[:, :, c0:c0 + n], in1=gatt.unsqueeze(2).broadcast_to([128, 8, n]), op=ALU.mult), deps=[c_gc])
                g2_ = P.op("pool", lambda e: e.tensor_tensor(out=attg[:, 8:16, 0:n], in0=convT[:, :, c0:c0 + n], in1=gconv.unsqueeze(2).broadcast_to([128, 8, n]), op=ALU.mult), deps=[c_gc])
                q1_ = P.op("pool", lambda e: e.tensor_tensor(out=sqb[:, 0:8, 0:n], in0=attT[:, :, c0:c0 + n], in1=attT[:, :, c0:c0 + n], op=ALU.mult))
                q2_ = P.op("pool", lambda e: e.tensor_tensor(out=sqb[:, 8:16, 0:n], in0=convT[:, :, c0:c0 + n], in1=convT[:, :, c0:c0 + n], op=ALU.mult))
                for part in range(2):
                    for kc in range(8):
                        mss = P.op("pe", lambda e, part=part, kc=kc: e.matmul(psb[0][0:n, part:part + 1], lhsT=sqb[:, part * 8 + kc, 0:n], rhs=ones_b[:, 0:1],
                                                                            start=(kc == 0), stop=(kc == 7)), deps=[q2_, c_ones], signal=(kc == 7 and part == 1))
                cps = P.op("dve", lambda e: e.tensor_copy(out=st2[0:n, 0:2], in_=psb[0][0:n, 0:2]), deps=[mss])
                r2_ = rstd_of(st2[0:n, 0:2], st2[0:n, 2:4], 1024, [cps])
                yield
                obank = Ring([(psb[2], psb[3]), (psb[4], psb[5])])
                hdone = None
                for cb in range(D // WB):
                    k, wb, wt = load_wb(wo_bf, cb)
                    ko, (pa, pc), odeps = obank.next()
                    for kc in range(16):
                        pb_ = pa if kc < 8 else pc
                        mo = P.op("pe", lambda e, pb_=pb_, kc=kc, wb=wb: e.matmul(pb_[0:n, 0:WB], lhsT=attg[:, kc, 0:n], rhs=wb[:, kc, :],
                                                                            start=(kc % 8 == 0), stop=(kc % 8 == 7)), deps=[wt, g2_, odeps], signal=(kc == 15))
                    wring.release(k, [mo])
                    cs_ = slice(cb * WB, (cb + 1) * WB)
                    e1_ = P.op("dve", lambda e, pa=pa, cs_=cs_: e.scalar_tensor_tensor(out=h1[0:n, cs_], in0=pa[0:n, 0:WB], scalar=st2[0:n, 2:3], in1=xt[0:n, cs_], op0=ALU.mult, op1=ALU.add), deps=[mo, r2_, lx, h1_free[0]])
                    hdone = P.op("dve", lambda e, pc=pc, cs_=cs_: e.scalar_tensor_tensor(out=h1[0:n, cs_], in0=pc[0:n, 0:WB], scalar=st2[0:n, 3:4], in1=h1[0:n, cs_], op0=ALU.mult, op1=ALU.add), deps=[e1_])
                    obank.release(ko, [hdone])
                    yield
                sq2 = P.op("act", lambda e: e.activation(out=xn2b[0:n], in_=h1[0:n], func=AF.Square, accum_out=st2[0:n, 4:5]), deps=[hdone])
                r3_ = rstd_of(st2[0:n, 4:5], st2[0:n, 5:6], D, [sq2])
                nm2 = P.op("dve", lambda e: e.scalar_tensor_tensor(out=xn2[0:n], in0=h1[0:n], scalar=st2[0:n, 5:6], in1=gffn_bc[0:n], op0=ALU.mult, op1=ALU.mult), deps=[r3_])
                nb2 = P.op("pool", lambda e: e.tensor_copy(out=xn2b[0:n], in_=xn2[0:n]), deps=[nm2])
                yield
                tbank = Ring([psb[0][:].bitcast(BF16), psb[1][:].bitcast(BF16)])
                xT_done = None
                for q4 in range(4):
                    kp, pst, pdeps = tbank.next()
                    for j in range(4):
                        kc = q4 * 4 + j
                        tt_ = P.op("pe", lambda e, pst=pst, kc=kc, j=j: e.transpose(out=pst[:, j * 128:j * 128 + n], in_=xn2b[0:n, kc * 128:(kc + 1) * 128],
                                                                               identity=ident[0:n, 0:n]), deps=[nb2, c_id, pdeps, cps], signal=(j == 3))
                    xT_done = P.op("act", lambda e, pst=pst, q4=q4: e.activation(out=xn2T[:, q4 * 4:q4 * 4 + 4, 0:n],
                                                                             in_=pst[:, 0:512].rearrange("p (a b) -> p a b", b=128)[:, :, 0:n], func=AF.Copy), deps=[tt_])
                    tbank.release(kp, [xT_done])
                    yield
                qbank = Ring([psb[2], psb[3], psb[4], psb[5]])
                q_done = None
                for cb in range(D // WB):
                    k, wb, wt = load_wb(wq_bf, cb)
                    for c4 in range(WB // 128):
                        cc = cb * (WB // 128) + c4
                        kq_, pq_, qdeps = qbank.next()
                        for kc in range(16):
                            mq = P.op("pe", lambda e, pq_=pq_, kc=kc, wb=wb, c4=c4: e.matmul(pq_[:, 0:n], lhsT=wb[:, kc, c4 * 128:(c4 + 1) * 128], rhs=xn2T[:, kc, 0:n],
                                                                                      start=(kc == 0), stop=(kc == 15)), deps=[wt, xT_done, qdeps, hdone], signal=(kc == 15))
                        q_done = P.op("act", lambda e, pq_=pq_, cc=cc: e.activation(out=qT[:, cc, 0:n], in_=pq_[:, 0:n], func=AF.Copy), deps=[mq])
                        qbank.release(kq_, [q_done])
                    wring.release(k, [mq])
                    yield
                sbank = Ring([psb[6], psb[7]])
                sc_done = None
                for c4g in range(4):
                    ksb, psc, sdeps = sbank.next()
                    for j in range(4):
                        cc = c4g * 4 + j
                        msc = P.op("pe", lambda e, psc=psc, cc=cc, j=j: e.matmul(psc[0:n, j * 128:(j + 1) * 128], lhsT=qT[:, cc, 0:n], rhs=(kaT if cc % 2 == 0 else kbT),
                                                                            start=True, stop=True), deps=[q_done, sdeps], signal=(j == 3))
                    sc_done = P.op("dve", lambda e, psc=psc, c4g=c4g: e.tensor_copy(out=sc[0:n, c4g * 4:c4g * 4 + 4, :], in_=psc[0:n, :].rearrange("p (a b) -> p a b", b=128)), deps=[msc])
                    sbank.release(ksb, [sc_done])
                    yield
                t_ = sc_done
                for cc in range(16):
                    t_ = P.op("dve", lambda e, cc=cc: e.max(out=v16[0:n, cc, 0:8], in_=sc[0:n, cc, :]), deps=[t_])
                    t_ = P.op("dve", lambda e, cc=cc: e.max_index(out=i16[0:n, cc, 0:8], in_max=v16[0:n, cc, 0:8], in_values=sc[0:n, cc, :]), deps=[t_])
                    t_ = P.op("dve", lambda e, cc=cc: e.match_replace(out=scw[0:n], in_to_replace=v16[0:n, cc, 0:8], in_values=sc[0:n, cc, :], imm_value=-1e30), deps=[t_])
                    t_ = P.op("dve", lambda e, cc=cc: e.max(out=v16[0:n, cc, 8:16], in_=scw[0:n]), deps=[t_])
                    t_ = P.op("dve", lambda e, cc=cc: e.max_index(out=i16[0:n, cc, 8:16], in_max=v16[0:n, cc, 8:16], in_values=scw[0:n]), deps=[t_])
                    yield
                t_ = P.op("dve", lambda e: e.tensor_copy(out=idxf[0:n], in_=i16[0:n]), deps=[t_])
                c3 = lambda ap: ap.rearrange("p (a b) -> p a b", b=16)
                for h in range(8):
                    t_ = P.op("dve", lambda e, h=h: e.tensor_tensor(out=c3(cand[0:n]), in0=v16[0:n, 2 * h, :].unsqueeze(2).broadcast_to([n, 16, 16]),
                                                                 in1=v16[0:n, 2 * h + 1, :].unsqueeze(1).broadcast_to([n, 16, 16]), op=ALU.add), deps=[t_])
                    t_ = P.op("dve", lambda e, h=h: e.scalar_tensor_tensor(out=c3(cid[0:n]), in0=idxf[0:n, 2 * h, :].unsqueeze(2).broadcast_to([n, 16, 16]), scalar=128.0,
                                                                        in1=idxf[0:n, 2 * h + 1, :].unsqueeze(1).broadcast_to([n, 16, 16]), op0=ALU.mult, op1=ALU.add), deps=[t_])
                    t_ = P.op("dve", lambda e, h=h: e.max(out=ts[0:n, h, 0:8], in_=cand[0:n]), deps=[t_])
                    t_ = P.op("dve", lambda e, h=h: e.max_index(out=pos_u[0:n, h, 0:8], in_max=ts[0:n, h, 0:8], in_values=cand[0:n]), deps=[t_])
                    t_ = P.op("dve", lambda e, h=h: e.match_replace(out=cw[0:n], in_to_replace=ts[0:n, h, 0:8], in_values=cand[0:n], imm_value=-1e30), deps=[t_])
                    t_ = P.op("dve", lambda e, h=h: e.max(out=ts[0:n, h, 8:16], in_=cw[0:n]), deps=[t_])
                    t_ = P.op("dve", lambda e, h=h: e.max_index(out=pos_u[0:n, h, 8:16], in_max=ts[0:n, h, 8:16], in_values=cw[0:n]), deps=[t_])
                    t_ = P.op("dve", lambda e, h=h: e.tensor_copy(out=posf[0:n, h, :], in_=pos_u[0:n, h, :]), deps=[t_])
                    yield
                    for k_ in range(16):
                        t_ = P.op("dve", lambda e, h=h, k_=k_: e.scalar_tensor_tensor(out=junk256[0:n], in0=iota_f[0:n], scalar=posf[0:n, h, k_:k_ + 1], in1=cid[0:n],
                                                                                   op0=ALU.is_equal, op1=ALU.mult, accum_out=eidf[0:n, h * 16 + k_:h * 16 + k_ + 1]), deps=[t_])
                        if k_ % 4 == 3:
                            yield
                t_ = P.op("dve", lambda e: e.tensor_scalar(out=eidf[0:n], in0=eidf[0:n], scalar1=0.0, scalar2=16383.0, op0=ALU.max, op1=ALU.min), deps=[t_])
                eid_done = P.op("dve", lambda e: e.tensor_copy(out=eid[0:n], in_=eidf[0:n]), deps=[t_])
                t_ = P.op("dve", lambda e: e.tensor_tensor(out=tsub[0:n], in0=ts[0:n], in1=ts[0:n, :, 0:1].broadcast_to([n, 8, 16]), op=ALU.subtract), deps=[eid_done])
                t_ = P.op("act", lambda e: e.activation(out=ge[0:n], in_=tsub[0:n], func=AF.Exp), deps=[t_])
                t_ = P.op("dve", lambda e: e.tensor_reduce(out=zs[0:n], in_=ge[0:n], axis=AX.X, op=ALU.add), deps=[t_])
                t_ = P.op("dve", lambda e: e.reciprocal(out=zs[0:n], in_=zs[0:n]), deps=[t_])
                P.op("dve", lambda e: e.tensor_tensor(out=gate[0:n], in0=ge[0:n], in1=zs[0:n].unsqueeze(2).broadcast_to([n, 8, 16]), op=ALU.mult), deps=[t_])
                yield

            def gather(i):
                n = 128 if i < 8 else NS
                c0 = i * 128
                h1 = h1s[i % 2]; xn2 = xn2s[i % 2]; eid = eids[i % 2]; gate = gates[i % 2]
                dst = yp[c0:c0 + 128, :] if i < 8 else ys
                gflat = gate.rearrange("p a b -> p (a b)")
                ini = P.op("act", lambda e: e.activation(out=accb[0:n], in_=h1[0:n], func=AF.Copy))
                h1_free[0] = ini
                d_ = None
                for s_ in range(128):
                    k, ub, gdeps = gring.next()
                    gt_ = P.dma("pool", gring.ds[k], lambda e, ub=ub, s_=s_: e.indirect_dma_start(
                        out=ub[0:n], out_offset=None, in_=eu_bf, in_offset=bass.IndirectOffsetOnAxis(ap=eid[0:n, s_:s_ + 1], axis=0)), deps=[gdeps])
                    d_ = P.op("dve", lambda e, ub=ub, s_=s_: e.scalar_tensor_tensor(out=ub[0:n], in0=ub[0:n], scalar=1.0, in1=xn2[0:n], op0=ALU.mult, op1=ALU.mult,
                                                                              accum_out=dots[0:n, s_:s_ + 1]), deps=[gt_])
                    gring.release(k, [d_])
                    if s_ % 4 == 3:
                        yield
                a_ = P.op("act", lambda e: e.activation(out=actv[0:n], in_=dots[0:n], func=AF.Gelu), deps=[d_])
                cf = P.op("dve", lambda e: e.tensor_tensor(out=coef[0:n], in0=actv[0:n], in1=gflat[0:n], op=ALU.mult), deps=[a_])
                for s_ in range(128):
                    k, vb, gdeps = gring.next()
                    gt_ = P.dma("pool", gring.ds[k], lambda e, vb=vb, s_=s_: e.indirect_dma_start(
                        out=vb[0:n], out_offset=None, in_=ev_bf, in_offset=bass.IndirectOffsetOnAxis(ap=eid[0:n, s_:s_ + 1], axis=0)), deps=[gdeps])
                    d_ = P.op("dve", lambda e, vb=vb, s_=s_: e.scalar_tensor_tensor(out=accb[0:n], in0=vb[0:n], scalar=coef[0:n, s_:s_ + 1], in1=accb[0:n],
                                                                              op0=ALU.mult, op1=ALU.add), deps=[gt_, cf, ini])
                    gring.release(k, [d_])
                    if s_ % 4 == 3:
                        yield
                sq3 = P.op("act", lambda e: e.activation(out=xn2[0:n], in_=accb[0:n], func=AF.Square, accum_out=st3[0:n, 0:1]), deps=[d_])
                r4_ = rstd_of(st3[0:n, 0:1], st3[0:n, 1:2], D, [sq3])
                y_ = P.op("dve", lambda e: e.scalar_tensor_tensor(out=accb[0:n], in0=accb[0:n], scalar=st3[0:n, 1:2], in1=gfin_bc[0:n], op0=ALU.mult, op1=ALU.mult), deps=[r4_])
                P.dma("sp", ds_y, lambda e: e.dma_start(out=dst, in_=accb[0:n]), deps=[y_])
                yield

            for _ in front(0):
                pass
            P.barrier()
            for i in range(ntiles):
                gens = [gather(i)] + ([front(i + 1)] if i + 1 < ntiles else [])
                while gens:
                    for gph in list(gens):
                        try:
                            next(gph)
                        except StopIteration:
                            gens.remove(gph)
                P.barrier()

        if 'dbg' in phases:
            P.dma("sp", ds_copy, lambda e: e.dma_start(out=dbg_att, in_=attT.rearrange("p a b -> p (a b)")))
            P.dma("sp", ds_copy, lambda e: e.dma_start(out=dbg_conv, in_=convT.rearrange("p a b -> p (a b)")))
        P.barrier()
        with nc.Block() as block:
            P.replay(block)
    return nc


def _alibi():
    n = 48
    i = np.arange(1, n + 1, dtype=np.float32)
    return np.exp2(-8.0 * i / n).astype(np.float32).reshape(3, 16)


def _etables(half):
    sl = _alibi()
    k = np.arange(128)[:, None]; q = np.arange(128)[None, :]
    out = []
    for g, dil in ((0, 1), (1, 4)):
        e = np.zeros((128, 16, 3, 128), np.float32)
        for h in range(16):
            s = sl[g, h]
            steps_prev = q - k + 128
            prev = np.where(steps_prev <= 128, np.exp(-s * (steps_prev * dil).astype(np.float32)), 0.0)
            steps_cur = q - k
            cur = np.where(steps_cur >= 0, np.exp(-s * (steps_cur * dil).astype(np.float32)), 0.0)
            e[:, h, 0] = prev if half == 1 else 0.0
            e[:, h, 1] = prev
            e[:, h, 2] = cur
        out.append(e.astype(ml_dtypes.bfloat16))
    e2 = np.zeros((128, 16, 64), np.float32)
    i = np.arange(128)[:, None]; j = np.arange(64)[None, :]
    for h in range(16):
        s = sl[2, h]
        if half == 1:
            steps = 64 + j - i
            e2[:, h] = np.where(steps >= 0, np.exp(-s * (steps * 16).astype(np.float32)), 0.0)
        else:
            steps = j - (i - 64)
            e2[:, h] = np.where((steps >= 0) & (i >= 64), np.exp(-s * (steps * 16).astype(np.float32)), 0.0)
    out.append(e2.astype(ml_dtypes.bfloat16))
    return out


def _sample_tables():
    sl = _alibi()
    es_main = np.zeros((128, 3, 8, 16), np.float32)
    es_tail = np.zeros((8, 3, 8, 16), np.float32)
    for g, dil in enumerate(DILS):
        n_prev = CACHE_ROWS[g]
        for t in range(8):
            r = t % dil
            for m in range(128):
                row = dil * m + r
                d = n_prev + t - row
                st = d // dil
                if d % dil == 0 and 0 <= st <= 128 and r < 8:
                    es_main[m, g, t] = np.exp(-sl[g] * np.float32(d))
            for tp in range(8):
                d = t - tp
                if d >= 0 and d % dil == 0 and d // dil <= 128:
                    es_tail[tp, g, t] = np.exp(-sl[g] * np.float32(d))
    return es_main, es_tail


_NC_CACHE = {}


def kernel(x_prompt, x_sample, cache_k_g0, cache_v_g0, cache_k_g1, cache_v_g1, cache_k_g2, cache_v_g2,
           state_conv, norm_mix, w_in, conv_w, norm_att_out, norm_conv_out, w_out, norm_ffn, w_pq,
           sub_keys_a, sub_keys_b, expert_u, expert_v, norm_final, _phases=PHASES):
    f = lambda a: np.ascontiguousarray(np.asarray(a, dtype=np.float32))
    x_prompt = f(x_prompt); x_sample = f(x_sample)
    cks = [f(cache_k_g0), f(cache_k_g1), f(cache_k_g2)]
    cvs = [f(cache_v_g0), f(cache_v_g1), f(cache_v_g2)]
    state_conv = f(state_conv)
    shared = dict(w_in=f(w_in)[0], conv_w=f(conv_w)[0], g_mix=f(norm_mix)[0], g_att=f(norm_att_out)[0],
                  g_conv=f(norm_conv_out)[0], w_out=f(w_out)[0], g_ffn=f(norm_ffn)[0], w_pq=f(w_pq)[0],
                  ska=f(sub_keys_a)[0], skb=f(sub_keys_b)[0], eu=f(expert_u)[0], ev=f(expert_v)[0], g_fin=f(norm_final),
                  ident_b=np.eye(128, dtype=np.float32).astype(ml_dtypes.bfloat16), ident_f=np.eye(128, dtype=np.float32))
    es_main, es_tail = _sample_tables()
    shared["es_main"] = es_main; shared["es_tail"] = es_tail
    selq = np.zeros((32, 32, 128), np.float32)
    for j in range(32):
        selq[j, j, :] = 1.0
    shared["selq"] = selq.astype(ml_dtypes.bfloat16)
    selo = np.zeros((128, 64), np.float32); selo[:, 31] = 1.0
    shared["selo"] = selo.astype(ml_dtypes.bfloat16)
    shared["iota256"] = np.arange(256, dtype=np.float32)
    et = [_etables(0), _etables(1)]
    in_maps = []
    for c in range(8):
        b, half = c // 2, c % 2
        if half == 1:
            xp = x_prompt[b]
        else:
            xp = np.concatenate([np.zeros((NOWN, D), np.float32), x_prompt[b, 0:NOWN]], axis=0)
        m = dict(shared)
        m["xp"] = np.ascontiguousarray(xp)
        m["xs"] = np.ascontiguousarray(x_sample[4 * c:4 * c + 4].reshape(NS, D))
        for g in range(3):
            m["ck%d" % g] = np.ascontiguousarray(cks[g][0, 4 * c:4 * c + 4].reshape(4, CACHE_ROWS[g], 1024))
            m["cv%d" % g] = np.ascontiguousarray(cvs[g][0, 4 * c:4 * c + 4].reshape(4, CACHE_ROWS[g], 1024))
        m["sconv"] = np.ascontiguousarray(state_conv[0, 4 * c:4 * c + 4])
        m["e0"], m["e1"], m["e2"] = et[half]
        in_maps.append(m)
    key = tuple(_phases)
    if key not in _NC_CACHE:
        _NC_CACHE[key] = build(_phases)
    nc = _NC_CACHE[key]
    res = run_bass_kernel_spmd(nc, in_maps, core_ids=list(range(8))).results
    y_prompt = np.zeros((4, 2048, D), np.float32)
    y_sample = np.zeros((32, 8, D), np.float32)
    nk_p = [np.zeros((1, 4, HALO[g] if g < 2 else 2048, 16, 64), np.float32) for g in range(3)]
    nv_p = [np.zeros((1, 4, HALO[g] if g < 2 else 2048, 16, 64), np.float32) for g in range(3)]
    nconv_p = np.zeros((1, 4, 2, 1024), np.float32)
    nk_s = [np.zeros((1, 32, CACHE_ROWS[g], 16, 64), np.float32) for g in range(3)]
    nv_s = [np.zeros((1, 32, CACHE_ROWS[g], 16, 64), np.float32) for g in range(3)]
    nconv_s = np.zeros((1, 32, 2, 1024), np.float32)
    for c in range(8):
        b, half = c // 2, c % 2
        r = res[c]
        y_prompt[b, half * NOWN:(half + 1) * NOWN] = r["yp"]
        y_sample[4 * c:4 * c + 4] = r["ys"].reshape(4, 8, D)
        if half == 1:
            for g in range(2):
                nk_p[g][0, b] = r["okp%d" % g].reshape(HALO[g], 16, 64)
                nv_p[g][0, b] = r["ovp%d" % g].reshape(HALO[g], 16, 64)
            nconv_p[0, b] = r["oconvp"]
        nk_p[2][0, b, half * NOWN:(half + 1) * NOWN] = r["okp2"].reshape(NOWN, 16, 64)
        nv_p[2][0, b, half * NOWN:(half + 1) * NOWN] = r["ovp2"].reshape(NOWN, 16, 64)
        for g in range(3):
            nk_s[g][0, 4 * c:4 * c + 4] = r["oks%d" % g].reshape(4, CACHE_ROWS[g], 16, 64)
            nv_s[g][0, 4 * c:4 * c + 4] = r["ovs%d" % g].reshape(4, CACHE_ROWS[g], 16, 64)
        nconv_s[0, 4 * c:4 * c + 4] = r["oconvs"]
    return (y_prompt, y_sample, nk_p[0], nv_p[0], nk_p[1], nv_p[1], nk_p[2], nv_p[2], nconv_p,
            nk_s[0], nv_s[0], nk_s[1], nv_s[1], nk_s[2], nv_s[2], nconv_s)
```

```python
import math
from contextlib import ExitStack
import numpy as np
import ml_dtypes
import concourse.bass as bass
import concourse.mybir as mybir
from concourse.bass_utils import run_bass_kernel_spmd

F32 = mybir.dt.float32
BF16 = mybir.dt.bfloat16
I32 = mybir.dt.int32
U32 = mybir.dt.uint32
AF = mybir.ActivationFunctionType
ALU = mybir.AluOpType
AX = mybir.AxisListType

D = 2048
NOWN = 1024
NCTX = 2048
NS = 32
NTOK = NCTX + NS
EPS = 1e-6
DILS = (1, 4, 16)
HALO = (128, 512, 1024)
CS = (NCTX - NOWN - 128, NCTX - NOWN - 512, 0)
CACHE_ROWS = (128, 512, 2048)
PHASES = ("A1", "A2", "A3", "A3s", "B")


class EngQ:
    def __init__(self, name, sem):
        self.name = name
        self.sem = sem
        self.cnt = 0
        self.ops = []
        self.seen = {}


class DSem:
    def __init__(self, sem):
        self.sem = sem
        self.cnt = 0


class Prog:
    def __init__(self, nc, sems, dsems):
        self.nc = nc
        self.q = {n: EngQ(n, sems[n]) for n in ("pe", "act", "dve", "pool", "sp")}
        self.dsems = [DSem(s) for s in dsems]
        self.nd = 0

    def new_dsem(self):
        d = self.dsems[self.nd]
        self.nd += 1
        return d

    def _waits(self, q, deps):
        waits = []
        for d in deps:
            if d is None:
                continue
            if isinstance(d, (list, tuple)) and (len(d) == 0 or not isinstance(d[0], (EngQ, DSem))):
                waits += self._waits(q, d)
                continue
            src, c = d
            if c <= 0:
                continue
            key = id(src)
            if q.seen.get(key, 0) >= c:
                continue
            q.seen[key] = c
            waits.append((src.sem, c))
        return waits

    def op(self, qn, fn, deps=(), signal=True):
        q = self.q[qn]
        waits = self._waits(q, deps)
        tok = None
        if signal:
            q.cnt += 1
            tok = (q, q.cnt)
        q.ops.append((waits, fn, q.sem if signal else None, 1))
        return tok

    def dma(self, qn, dsem, fn, deps=()):
        q = self.q[qn]
        waits = self._waits(q, deps)
        dsem.cnt += 16
        q.ops.append((waits, fn, dsem.sem, 16))
        return (dsem, dsem.cnt)

    def all_tokens(self):
        return [(q, q.cnt) for q in self.q.values() if q.name != "sp"] + [(d, d.cnt) for d in self.dsems]

    def barrier(self):
        toks = self.all_tokens()
        for q in self.q.values():
            waits = self._waits(q, toks)
            q.ops.append((waits, None, None, 0))

    def replay(self, block):
        def run(q):
            def f(eng):
                for waits, fn, sem, inc in q.ops:
                    for s, c in waits:
                        eng.wait_ge(s, c)
                    if fn is None:
                        continue
                    ins = fn(eng)
                    if sem is not None:
                        ins.then_inc(sem, inc)
            return f
        block.tensor(run(self.q["pe"]))
        block.scalar(run(self.q["act"]))
        block.vector(run(self.q["dve"]))
        block.gpsimd(run(self.q["pool"]))
        block.sync(run(self.q["sp"]))


class Arena:
    def __init__(self, nc, es, nbytes):
        self.t = es.enter_context(nc.sbuf_tensor("arena", [128, nbytes // 4], F32))
        self.off = 0
        self.cap = nbytes

    def alloc(self, free_shape, dt):
        n = 1
        for s in free_shape:
            n *= s
        esz = 4 if dt in (F32, I32, U32) else 2
        nb = (n * esz + 63) // 64 * 64
        assert self.off + nb <= self.cap, ("SBUF arena overflow", self.off, nb, self.cap)
        a = self.t[:, self.off // 4:(self.off + nb) // 4]
        self.off += nb
        if dt != F32:
            a = a.bitcast(dt)
        a = a[:, 0:n]
        if len(free_shape) == 2:
            a = a.rearrange("p (a b) -> p a b", b=free_shape[1])
        elif len(free_shape) == 3:
            a = a.rearrange("p (a b c) -> p a b c", b=free_shape[1], c=free_shape[2])
        return a


class Ring:
    def __init__(self, bufs, P=None):
        self.bufs = bufs
        self.free = [[] for _ in bufs]
        self.i = 0
        self.ds = [P.new_dsem() for _ in bufs] if P is not None else None

    def next(self):
        k = self.i % len(self.bufs)
        self.i += 1
        deps = self.free[k]
        self.free[k] = []
        return k, self.bufs[k], deps

    def release(self, k, toks):
        self.free[k] += [t for t in toks if t is not None]


def ss(start, n, step):
    return slice(start, start + (n - 1) * step + 1, step)


def build(phases=PHASES):
    nc = bass.Bass("TRN2", target_bir_lowering=False)

    def din(name, shape, dt=F32):
        return nc.dram_tensor(name, list(shape), dt, kind="ExternalInput").ap()

    def dout(name, shape, dt=F32):
        return nc.dram_tensor(name, list(shape), dt, kind="ExternalOutput").ap()

    xp = din("xp", [NCTX, D]); xs = din("xs", [NS, D])
    w_in = din("w_in", [D, 12288]); conv_w = din("conv_w", [3, 1024])
    g_mix = din("g_mix", [D]); g_att = din("g_att", [1024]); g_conv = din("g_conv", [1024])
    w_out = din("w_out", [D, D]); g_ffn = din("g_ffn", [D]); w_pq = din("w_pq", [D, D])
    ska = din("ska", [128, 128]); skb = din("skb", [128, 128])
    if "B" in phases:
        eu = din("eu", [16384, D]); ev = din("ev", [16384, D])
    g_fin = din("g_fin", [D])
    ck = [din("ck%d" % g, [4, CACHE_ROWS[g], 1024]) for g in range(3)]
    cv = [din("cv%d" % g, [4, CACHE_ROWS[g], 1024]) for g in range(3)]
    sconv = din("sconv", [4, 2, 1024])
    e0 = din("e0", [128, 16, 3, 128], BF16); e1 = din("e1", [128, 16, 3, 128], BF16)
    e2 = din("e2", [128, 16, 64], BF16)
    ident_b = din("ident_b", [128, 128], BF16); ident_f = din("ident_f", [128, 128])
    es_main = din("es_main", [128, 3, 8, 16]); es_tail = din("es_tail", [8, 3, 8, 16])
    selq = din("selq", [32, 32, 128], BF16); selo = din("selo", [128, 64], BF16)
    iota_d = din("iota256", [256])
    sel4_d = din("sel4", [128, 32])

    if "B" in phases:
        eu_bf = nc.dram_tensor("eu_bf", [16384, D], BF16, kind="Internal").ap()
        ev_bf = nc.dram_tensor("ev_bf", [16384, D], BF16, kind="Internal").ap()
        wo_bf = nc.dram_tensor("wo_bf", [D, D], BF16, kind="Internal").ap()
        wq_bf = nc.dram_tensor("wq_bf", [D, D], BF16, kind="Internal").ap()
    yp = dout("yp", [NOWN, D]); ys = dout("ys", [NS, D])
    okp = [dout("okp%d" % g, [HALO[g], 1024]) for g in range(3)]
    ovp = [dout("ovp%d" % g, [HALO[g], 1024]) for g in range(3)]
    oconvp = dout("oconvp", [2, 1024])
    oks = [dout("oks%d" % g, [4, CACHE_ROWS[g], 1024]) for g in range(3)]
    ovs = [dout("ovs%d" % g, [4, CACHE_ROWS[g], 1024]) for g in range(3)]
    oconvs = dout("oconvs", [4, 2, 1024])
    if 'dbg' in phases:
        dbg_ofin = dout("dbg_ofin", [8, NS, 130]); dbg_atts = dout("dbg_atts", [8, NS, 128], BF16)
        dbg_att = dout("dbg_att", [128, 8 * (NOWN + NS)], BF16); dbg_conv = dout("dbg_conv", [128, 8 * (NOWN + NS)], BF16)

    with ExitStack() as es:
        ar = Arena(nc, es, 190 * 1024)
        psb = [es.enter_context(nc.psum_tensor("ps%d" % i, [128, 512], F32)) for i in range(8)]
        sems = {n: es.enter_context(nc.semaphore("s_" + n)) for n in ("pe", "act", "dve", "pool", "sp")}
        dsems = [es.enter_context(nc.semaphore("d%d" % i)) for i in range(28)]
        P = Prog(nc, sems, dsems)
        ds_const = P.new_dsem(); ds_c2 = P.new_dsem(); ds_e = [P.new_dsem(), P.new_dsem()]
        ds_copy = P.new_dsem(); ds_c = P.new_dsem(); ds_cv = P.new_dsem()
        out_tokens = []

        ident = ar.alloc([128], BF16)
        identf = ar.alloc([128], F32)
        ones_b = ar.alloc([128], BF16)
        attT = ar.alloc([8, NOWN + NS], BF16)
        convT = ar.alloc([8, NOWN + NS], BF16)
        gatt = ar.alloc([8], F32); gconv = ar.alloc([8], F32)
        c_id = P.dma("sp", ds_const, lambda e: e.dma_start(out=ident, in_=ident_b))
        c_idf = P.dma("sp", ds_const, lambda e: e.dma_start(out=identf, in_=ident_f))
        c_ga = P.dma("sp", ds_const, lambda e: e.dma_start(out=gatt, in_=g_att.rearrange("(j c) -> c j", c=128), allow_slow_non_contiguous=True))
        c_gc = P.dma("sp", ds_const, lambda e: e.dma_start(out=gconv, in_=g_conv.rearrange("(j c) -> c j", c=128), allow_slow_non_contiguous=True))
        c_ones = P.op("pool", lambda e: e.memset(ones_b, 1.0))
        epsc = ar.alloc([1], F32)
        c_eps = P.op("pool", lambda e: e.memset(epsc, EPS))
        c_id = c_gc; c_idf = c_gc; c_ga = c_gc

        if "A3" in phases:
            for g in range(3):
                R = CACHE_ROWS[g]
                for b in range(4):
                    for src, dst in ((ck[g], oks[g]), (cv[g], ovs[g])):
                        out_tokens.append(P.dma("sp", ds_copy, lambda e, s=src, d=dst, b=b, R=R: e.dma_start(
                            out=d[b, 0:R - 8, :], in_=s[b, 8:R, :])))

        mark_persist = ar.off

        nd_persist = P.nd
        xnT = ar.alloc([16, NTOK], BF16)
        mark_xn = ar.off
        gmix_bc = ar.alloc([D], F32)
        xt_ring = Ring([ar.alloc([D], F32) for _ in range(2)], P)
        xnb_ring = Ring([ar.alloc([D], BF16) for _ in range(2)])
        junk_b = ar.alloc([D], BF16)
        stat = ar.alloc([64], F32)
        c_gm = P.dma("sp", ds_c2, lambda e: e.dma_start(out=gmix_bc, in_=g_mix.partition_broadcast(128)))
        psT_ring = Ring([psb[0][:].bitcast(BF16), psb[1][:].bitcast(BF16)])
        xn_ready = []
        for i in range(17):
            n = 128 if i < 16 else NS
            src = xp[i * 128:(i + 1) * 128, :] if i < 16 else xs
            k, xt, fdeps = xt_ring.next()
            ld = P.dma("sp", xt_ring.ds[k], lambda e, xt=xt, src=src, n=n: e.dma_start(out=xt[0:n], in_=src), deps=fdeps)
            sq = P.op("act", lambda e, xt=xt, n=n, i=i: e.activation(out=junk_b[0:n], in_=xt[0:n], func=AF.Square,
                                                                  accum_out=stat[0:n, i:i + 1]), deps=[ld])
            r1 = P.op("act", lambda e, n=n, i=i: e.activation(out=stat[0:n, 32 + i:33 + i], in_=stat[0:n, i:i + 1], func=AF.Sqrt,
                                                              scale=1.0 / D, bias=epsc[0:n, 0:1]), deps=[sq, c_eps])
            r2 = P.op("dve", lambda e, n=n, i=i: e.reciprocal(out=stat[0:n, 32 + i:33 + i], in_=stat[0:n, 32 + i:33 + i]), deps=[r1])
            kb, xnb, bdeps = xnb_ring.next()
            nm = P.op("dve", lambda e, xt=xt, xnb=xnb, n=n, i=i: e.scalar_tensor_tensor(
                out=xnb[0:n], in0=xt[0:n], scalar=stat[0:n, 32 + i:33 + i], in1=gmix_bc[0:n], op0=ALU.mult, op1=ALU.mult),
                deps=[r2, c_gm, bdeps])
            xt_ring.release(k, [nm, sq])
            tl = []
            for q4 in range(4):
                kp, pst, pdeps = psT_ring.next()
                for j in range(4):
                    kc = q4 * 4 + j
                    t = P.op("pe", lambda e, pst=pst, xnb=xnb, kc=kc, j=j, n=n: e.transpose(
                        out=pst[:, j * 128:j * 128 + n], in_=xnb[0:n, kc * 128:(kc + 1) * 128], identity=ident[0:n, 0:n]),
                        deps=[nm, c_id, pdeps], signal=(j == 3))
                c0 = i * 128
                cp = P.op("act", lambda e, pst=pst, q4=q4, c0=c0, n=n: e.activation(
                    out=xnT[:, q4 * 4:q4 * 4 + 4, c0:c0 + n],
                    in_=pst[:, 0:512].rearrange("p (a b) -> p a b", b=128)[:, :, 0:n], func=AF.Copy), deps=[t])
                psT_ring.release(kp, [cp])
                tl.append(cp)
            xnb_ring.release(kb, [t])
            xn_ready.append(tl)
        xn_all = [tl[-1] for tl in xn_ready]
        P.barrier()
        ar.off = mark_xn
        P.nd = nd_persist

        w_ring = Ring([ar.alloc([16, 128], BF16) for _ in range(3)], P)

        def load_w(col0):
            conv_step()
            k, wb, fdeps = w_ring.next()
            t = P.dma("pool", w_ring.ds[k], lambda e, wb=wb, col0=col0: e.dma_start(
                out=wb, in_=w_in[:, col0:col0 + 128].rearrange("(kc p) c -> p kc c", p=128)), deps=fdeps)
            return k, wb, t

        acc_ring = Ring([psb[0], psb[1]])
        conv_state = {"k": 0}
        mark_conv = ar.off
        ds_conv = [P.new_dsem(), P.new_dsem()]
        conv_tok = [None, None]

        def conv_step():
            k = conv_state["k"]
            if "B" not in phases or k >= 36:
                return
            conv_state["k"] = k + 1
            if k < 32:
                tab, tabb = (eu, eu_bf) if k < 16 else (ev, ev_bf)
                r0 = (k % 16) * 1024
            else:
                tab, tabb = (w_out, wo_bf) if k < 34 else (w_pq, wq_bf)
                r0 = (k % 2) * 1024
            conv_tok[k % 2] = P.dma("pool", ds_conv[k % 2], lambda e, tab=tab, tabb=tabb, r0=r0: e.dma_start(
                out=tabb[r0:r0 + 1024, :], in_=tab[r0:r0 + 1024, :]), deps=[conv_tok[k % 2]])

        mark_a = ar.off
        nd_a = P.nd

        if "A1" in phases:
            KT = [ar.alloc([NCTX - CS[g]], BF16) for g in range(3)]
            QT = [ar.alloc([NOWN], BF16) for g in range(3)]
            NVT = (9, 12, 16)
            VT = [ar.alloc([NVT[g], 128], BF16) for g in range(3)]
            Uacc = ar.alloc([NOWN], F32); Lacc = ar.alloc([NOWN], F32)
            vst_ring = Ring([ar.alloc([128], F32) for _ in range(2)], P)
            kst_ring = Ring([ar.alloc([128], F32) for _ in range(2)], P)
            e0p = [ar.alloc([2, 3, 128], BF16) for _ in range(2)]
            e1p = [ar.alloc([2, 3, 128], BF16) for _ in range(2)]
            e2p = [ar.alloc([2, 64], BF16) for _ in range(2)]
            pT_ring = [Ring([ar.alloc([256], BF16) for _ in range(2)]) for _ in range(2)]
            psS_ring = [Ring([psb[2 + 2 * h][:, 0:256], psb[3 + 2 * h][:, 0:256]]) for h in range(2)]
            psUL_ring = Ring([psb[6], psb[7]])
            e_free = [[], []]
            kq_free = []
            acc_free = []
            for p in range(8):
                es_ = p % 2
                ed = e_free[es_]; e_free[es_] = []
                te = [P.dma("sp", ds_e[es_], lambda e, p=p, s=es_: e.dma_start(out=e0p[s], in_=e0[:, 2 * p:2 * p + 2]), deps=ed),
                      P.dma("sp", ds_e[es_], lambda e, p=p, s=es_: e.dma_start(out=e1p[s], in_=e1[:, 2 * p:2 * p + 2]), deps=ed),
                      P.dma("sp", ds_e[es_], lambda e, p=p, s=es_: e.dma_start(out=e2p[s], in_=e2[:, 2 * p:2 * p + 2]), deps=ed)]
                te = te[-1]
                if 'noe' in phases:
                    pass
                kq_ready = [[], [], []]
                v_ready = [dict() for _ in range(3)]
                new_kq_free = []
                for g in range(3):
                    dil = DILS[g]
                    nctx = NCTX - CS[g]
                    k, wb, wt = load_w(3072 + g * 1024 + p * 128)
                    last = None
                    for t0 in range(0, nctx, 512):
                        n = min(512, nctx - t0)
                        ka, pacc, adeps = acc_ring.next()
                        for kc in range(16):
                            last = P.op("pe", lambda e, pacc=pacc, wb=wb, kc=kc, t0=t0, n=n, g=g: e.matmul(
                                pacc[:, 0:n], lhsT=wb[:, kc, :], rhs=xnT[:, kc, CS[g] + t0:CS[g] + t0 + n],
                                start=(kc == 0), stop=(kc == 15)), deps=[wt, adeps, xn_all], signal=(kc == 15))
                        cp = P.op("act", lambda e, pacc=pacc, t0=t0, n=n, g=g: e.activation(
                            out=KT[g][:, t0:t0 + n], in_=pacc[:, 0:n], func=AF.Copy), deps=[last, kq_free])
                        acc_ring.release(ka, [cp])
                        kq_ready[g].append(cp)
                    w_ring.release(k, [last])
                    nkt = HALO[g] // 128 if g < 2 else 8
                    for ti in range(nkt if 'nokout' not in phases else 0):
                        c0 = nctx - nkt * 128 + ti * 128
                        kk, pkb, kdeps = acc_ring.next()
                        pk = pkb[:].bitcast(BF16)[:, 0:128]
                        tr = P.op("pe", lambda e, pk=pk, c0=c0, g=g: e.transpose(out=pk, in_=KT[g][:, c0:c0 + 128], identity=ident),
                                  deps=[kq_ready[g][-1], kdeps])
                        ks, kst, sdeps = kst_ring.next()
                        cpk = P.op("dve", lambda e, pk=pk, kst=kst: e.tensor_copy(out=kst, in_=pk), deps=[tr, sdeps])
                        acc_ring.release(kk, [cpk])
                        do = P.dma("sp", kst_ring.ds[ks], lambda e, kst=kst, g=g, ti=ti, p=p: e.dma_start(
                            out=okp[g][ti * 128:(ti + 1) * 128, p * 128:(p + 1) * 128], in_=kst), deps=[cpk])
                        kst_ring.release(ks, [do])
                        out_tokens.append(do)
                        new_kq_free.append(tr)
                    k, wb, wt = load_w(g * 1024 + p * 128)
                    for t0 in range(0, NOWN, 512):
                        ka, pacc, adeps = acc_ring.next()
                        for kc in range(16):
                            last = P.op("pe", lambda e, pacc=pacc, wb=wb, kc=kc, t0=t0: e.matmul(
                                pacc[:, 0:512], lhsT=wb[:, kc, :], rhs=xnT[:, kc, NCTX - NOWN + t0:NCTX - NOWN + t0 + 512],
                                start=(kc == 0), stop=(kc == 15)), deps=[wt, adeps], signal=(kc == 15))
                        cp = P.op("act", lambda e, pacc=pacc, t0=t0, g=g: e.activation(
                            out=QT[g][:, t0:t0 + 512], in_=pacc[:, 0:512], func=AF.Copy), deps=[last, kq_free])
                        acc_ring.release(ka, [cp])
                        kq_ready[g].append(cp)
                    w_ring.release(k, [last])
                    k, wb, wt = load_w(6144 + g * 1024 + p * 128)
                    nblk = nctx // (128 * dil)
                    for r in range(dil if ('nov' not in phases and not ('vg0' in phases and g > 0) and not ('vg1' in phases and g != 1)) else 0):
                        for blk in range(nblk):
                            vi = r * nblk + blk
                            tstart = CS[g] + blk * 128 * dil + r
                            kv, pvb, vdeps = acc_ring.next()
                            pv = pvb[:, 0:128]
                            for kc in range(16):
                                last = P.op("pe", lambda e, pv=pv, wb=wb, kc=kc, tstart=tstart, dil=dil: e.matmul(
                                    pv, lhsT=xnT[:, kc, ss(tstart, 128, dil)], rhs=wb[:, kc, :],
                                    start=(kc == 0), stop=(kc == 15)), deps=[wt, vdeps], signal=(kc == 15))
                            if g == 0 and blk == nblk - 1:
                                rows = (0, 128, 1, 0)
                            elif g == 1 and blk == nblk - 1:
                                rows = (r, 512, 4, 0)
                            elif g == 2:
                                rows = (r, 1024, 16, 64)
                            else:
                                rows = None
                            if rows is not None and 'novout' not in phases:
                                r0, rn, rs, p0 = rows
                                ks, vst, sdeps = vst_ring.next()
                                cpo = P.op("act", lambda e, pv=pv, vst=vst: e.activation(out=vst, in_=pv, func=AF.Copy),
                                           deps=[last, sdeps])
                                cpv = P.op("dve", lambda e, vst=vst, g=g, vi=vi: e.tensor_copy(out=VT[g][:, vi, :], in_=vst),
                                           deps=[cpo, kq_free])
                                do = P.dma("sp", vst_ring.ds[ks], lambda e, vst=vst, g=g, p=p, r0=r0, rn=rn, rs=rs, p0=p0: e.dma_start(
                                    out=ovp[g][r0:rn:rs, p * 128:(p + 1) * 128], in_=vst[p0:128, :]), deps=[cpo])
                                vst_ring.release(ks, [do, cpv])
                                out_tokens.append(do)
                                rel = [cpo]
                            else:
                                cpv = P.op("dve", lambda e, pv=pv, g=g, vi=vi: e.tensor_copy(out=VT[g][:, vi, :], in_=pv),
                                           deps=[last, kq_free])
                                rel = [cpv]
                            v_ready[g][vi] = cpv
                            acc_ring.release(kv, rel)
                    w_ring.release(k, [last])

                units = []
                for n in range(8):
                    units.append(dict(g=0, nq=128, q=slice(n * 128, n * 128 + 128), o=slice(n * 128, n * 128 + 128),
                                      kts=[(slice(n * 128, n * 128 + 128), n, e0p[es_][:, :, 0 if n == 0 else 1, :]),
                                           (slice((n + 1) * 128, (n + 2) * 128), n + 1, e0p[es_][:, :, 2, :])], first=True))
                for r in range(4):
                    for n in (1, 2):
                        qs = ss((n - 1) * 512 + r, 128, 4)
                        units.append(dict(g=1, nq=128, q=qs, o=qs,
                                          kts=[(ss((n - 1) * 512 + r, 128, 4), r * 3 + n - 1,
                                                e1p[es_][:, :, 0 if n == 1 else 1, :]),
                                               (ss(n * 512 + r, 128, 4), r * 3 + n, e1p[es_][:, :, 2, :])],
                                          first=False))
                for r in range(16):
                    qs = slice(r, 1024, 16)
                    units.append(dict(g=2, nq=64, q=qs, o=qs, kts=[(slice(r, 2048, 16), r, e2p[es_])], first=False))
                last_mask = None
                if 'noattn' in phases:
                    units = []
                    a1 = a2 = None
                def stage_s(u):
                    g = u["g"]; nq = u["nq"]; nk = len(u["kts"])
                    pts = []
                    mk = None
                    for h in range(2):
                        hs = slice(h * 64, h * 64 + 64)
                        ksl, psS, sdeps = psS_ring[h].next()
                        for ki, (kcols, vi, etab) in enumerate(u["kts"]):
                            mm = P.op("pe", lambda e, psS=psS, g=g, hs=hs, kcols=kcols, q=u["q"], ki=ki, nq=nq: e.matmul(
                                psS[:, ki * 128:ki * 128 + nq], lhsT=KT[g][hs, kcols], rhs=QT[g][hs, q], start=True, stop=True),
                                deps=[kq_ready[g], sdeps], signal=(ki == nk - 1))
                        kp, pT, pdeps = pT_ring[h].next()
                        ex = P.op("act", lambda e, psS=psS, pT=pT, nk=nk, nq=nq: e.activation(
                            out=pT[:, 0:nk * 128].rearrange("p (a b) -> p a b", b=128)[:, :, 0:nq],
                            in_=psS[:, 0:nk * 128].rearrange("p (a b) -> p a b", b=128)[:, :, 0:nq], func=AF.Exp, scale=0.125),
                            deps=[mm, pdeps])
                        psS_ring[h].release(ksl, [ex])
                        for ki, (kcols, vi, etab) in enumerate(u["kts"]):
                            mk = P.op("pool", lambda e, pT=pT, ki=ki, nq=nq, etab=etab, h=h: e.tensor_tensor(
                                out=pT[:, ki * 128:ki * 128 + nq], in0=pT[:, ki * 128:ki * 128 + nq], in1=etab[:, h, 0:nq], op=ALU.mult),
                                deps=[ex, te])
                        pts.append((kp, pT, mk))
                    u["pts"] = pts
                    return mk

                def stage_pv(u):
                    g = u["g"]; nq = u["nq"]; nk = len(u["kts"])
                    ku, pUL, udeps = psUL_ring.next()
                    pU = pUL[:, 0:128]; pL = pUL[:, 128:256]
                    ml = None
                    for h in range(2):
                        hs = slice(h * 64, h * 64 + 64)
                        kp, pT, mk = u["pts"][h]
                        for ki, (kcols, vi, etab) in enumerate(u["kts"]):
                            P.op("pe", lambda e, pU=pU, hs=hs, g=g, vi=vi, pT=pT, ki=ki, nq=nq, nk=nk: e.matmul(
                                pU[hs, 0:nq], lhsT=VT[g][:, vi, hs], rhs=pT[:, ki * 128:ki * 128 + nq], start=(ki == 0), stop=(ki == nk - 1)),
                                deps=[mk, v_ready[g][vi], udeps], signal=False)
                        for ki, (kcols, vi, etab) in enumerate(u["kts"]):
                            ml = P.op("pe", lambda e, pL=pL, hs=hs, pT=pT, ki=ki, nq=nq, nk=nk: e.matmul(
                                pL[hs, 0:nq], lhsT=ones_b[:, 0:64], rhs=pT[:, ki * 128:ki * 128 + nq], start=(ki == 0), stop=(ki == nk - 1)),
                                deps=[c_ones], signal=(ki == nk - 1))
                        pT_ring[h].release(kp, [ml])
                    if u["first"]:
                        a1 = P.op("dve", lambda e, pU=pU, o=u["o"], nq=nq: e.tensor_copy(out=Uacc[:, o], in_=pU[:, 0:nq]), deps=[ml, acc_free])
                        a2 = P.op("dve", lambda e, pL=pL, o=u["o"], nq=nq: e.tensor_copy(out=Lacc[:, o], in_=pL[:, 0:nq]), deps=[ml, acc_free])
                    else:
                        a1 = P.op("dve", lambda e, pU=pU, o=u["o"], nq=nq: e.tensor_tensor(out=Uacc[:, o], in0=Uacc[:, o], in1=pU[:, 0:nq], op=ALU.add), deps=[ml])
                        a2 = P.op("dve", lambda e, pL=pL, o=u["o"], nq=nq: e.tensor_tensor(out=Lacc[:, o], in0=Lacc[:, o], in1=pL[:, 0:nq], op=ALU.add), deps=[ml])
                    psUL_ring.release(ku, [a1, a2])
                    new_kq_free.append(ml)
                    return a1, a2

                for ui in range(len(units) + 1):
                    if ui < len(units):
                        last_mask = stage_s(units[ui])
                    if ui >= 1:
                        a1, a2 = stage_pv(units[ui - 1])
                e_free[es_] = [last_mask]
                if 'nofin' in phases:
                    continue
                f1 = P.op("dve", lambda e: e.reciprocal(out=Lacc, in_=Lacc), deps=[a1, a2])
                f2 = P.op("dve", lambda e, p=p: e.tensor_tensor(out=attT[:, p, 0:NOWN], in0=Uacc, in1=Lacc, op=ALU.mult), deps=[f1])
                acc_free = [f2]
                kq_free = new_kq_free
            P.barrier()
        ar.off = mark_a
        P.nd = nd_a

        if "A2" in phases:
            NL = NOWN + 2 + NS
            C0 = NCTX - NOWN - 2
            gcS = ar.alloc([NL], F32); gbS = ar.alloc([NL], F32); uT = ar.alloc([NL], F32)
            cy = ar.alloc([NOWN], F32); cys = ar.alloc([4, 8], F32)
            ucat = ar.alloc([4, 10], F32)
            wc = ar.alloc([8, 3], F32)
            for kk_ in range(3):
                c_wc = P.dma("sp", ds_c2, lambda e, kk_=kk_: e.dma_start(out=wc[:, :, kk_], in_=conv_w[kk_].rearrange("(j c) -> c j", c=128), allow_slow_non_contiguous=True))
            blocks = [(0, 512), (512, 512), (1024, NL - 1024)]
            cacc = [Ring([psb[2 * t], psb[2 * t + 1]]) for t in range(3)]
            prev = []
            for j in range(8):
                tiles = {}
                for wi, col in enumerate((9216, 10240, 11264)):
                    k, wb, wt = load_w(col + j * 128)
                    for bi, (t0, n) in enumerate(blocks):
                        ka, pacc, adeps = cacc[bi].next()
                        for kc in range(16):
                            last = P.op("pe", lambda e, pacc=pacc, wb=wb, kc=kc, t0=t0, n=n: e.matmul(
                                pacc[:, 0:n], lhsT=wb[:, kc, :], rhs=xnT[:, kc, C0 + t0:C0 + t0 + n], start=(kc == 0), stop=(kc == 15)),
                                deps=[wt, adeps, xn_all], signal=(kc == 15))
                        if wi == 0:
                            cp = P.op("act", lambda e, pacc=pacc, t0=t0, n=n: e.activation(out=gbS[:, t0:t0 + n], in_=pacc[:, 0:n], func=AF.Copy), deps=[last, prev])
                        elif wi == 1:
                            cp = P.op("act", lambda e, pacc=pacc, t0=t0, n=n: e.activation(out=gcS[:, t0:t0 + n], in_=pacc[:, 0:n], func=AF.Copy), deps=[last, prev])
                        else:
                            cp = P.op("dve", lambda e, pacc=pacc, t0=t0, n=n: e.tensor_tensor(out=uT[:, t0:t0 + n], in0=pacc[:, 0:n], in1=gcS[:, t0:t0 + n], op=ALU.mult),
                                      deps=[last, tiles[(1, bi)], prev])
                        tiles[(wi, bi)] = cp
                        cacc[bi].release(ka, [cp])
                    w_ring.release(k, [last])
                rdy = list(tiles.values())
                o1 = P.op("dve", lambda e, j=j: e.tensor_scalar(out=cy, in0=uT[:, 0:NOWN], scalar1=wc[:, j, 0:1], scalar2=None, op0=ALU.mult), deps=[rdy, c_wc])
                o2 = P.op("dve", lambda e, j=j: e.scalar_tensor_tensor(out=cy, in0=uT[:, 1:NOWN + 1], scalar=wc[:, j, 1:2], in1=cy, op0=ALU.mult, op1=ALU.add), deps=[o1])
                o3 = P.op("dve", lambda e, j=j: e.scalar_tensor_tensor(out=cy, in0=uT[:, 2:NOWN + 2], scalar=wc[:, j, 2:3], in1=cy, op0=ALU.mult, op1=ALU.add), deps=[o2])
                o4 = P.op("dve", lambda e, j=j: e.tensor_tensor(out=convT[:, j, 0:NOWN], in0=cy, in1=gbS[:, 2:NOWN + 2], op=ALU.mult), deps=[o3])
                for tt in range(2):
                    ls = P.dma("sp", ds_c, lambda e, j=j, tt=tt: e.dma_start(out=ucat[:, :, tt], in_=sconv[:, tt, j * 128:(j + 1) * 128].rearrange("b c -> c b"), allow_slow_non_contiguous=True), deps=[prev])
                s0 = P.op("dve", lambda e: e.tensor_copy(out=ucat[:, :, 2:10], in_=uT[:, NOWN + 2:NL].rearrange("p (b t) -> p b t", t=8)), deps=[rdy, prev])
                s1 = P.op("dve", lambda e, j=j: e.tensor_scalar(out=cys, in0=ucat[:, :, 0:8], scalar1=wc[:, j, 0:1], scalar2=None, op0=ALU.mult), deps=[s0, ls])
                s2 = P.op("dve", lambda e, j=j: e.scalar_tensor_tensor(out=cys, in0=ucat[:, :, 1:9], scalar=wc[:, j, 1:2], in1=cys, op0=ALU.mult, op1=ALU.add), deps=[s1])
                s3 = P.op("dve", lambda e, j=j: e.scalar_tensor_tensor(out=cys, in0=ucat[:, :, 2:10], scalar=wc[:, j, 2:3], in1=cys, op0=ALU.mult, op1=ALU.add), deps=[s2])
                s4 = P.op("dve", lambda e, j=j: e.tensor_tensor(out=convT[:, j, NOWN:NOWN + NS].rearrange("p (b t) -> p b t", t=8), in0=cys,
                                                               in1=gbS[:, NOWN + 2:NL].rearrange("p (b t) -> p b t", t=8), op=ALU.mult), deps=[s3])
                d1 = P.dma("sp", ds_cv, lambda e, j=j: e.dma_start(out=oconvp[:, j * 128:(j + 1) * 128].rearrange("t c -> c t"), in_=uT[:, NOWN:NOWN + 2], allow_slow_non_contiguous=True), deps=[rdy])
                for tt in range(2):
                    d2 = P.dma("sp", ds_cv, lambda e, j=j, tt=tt: e.dma_start(out=oconvs[:, tt, j * 128:(j + 1) * 128].rearrange("b c -> c b"), in_=ucat[:, :, 8 + tt], allow_slow_non_contiguous=True), deps=[s0])
                out_tokens += [d1, d2]
                prev = [o4, s4, d2, o3, s3]
            P.barrier()
        ar.off = mark_a
        P.nd = nd_a

        if "A3s" in phases:
            ar.off = mark_conv
            selq_sb = ar.alloc([32, 128], BF16); selo_sb = ar.alloc([64], BF16)
            esm = ar.alloc([3, 8, 16], F32); est = ar.alloc([3, 8, 16], F32)
            P.dma("sp", ds_c2, lambda e: e.dma_start(out=selq_sb[0:32], in_=selq))
            P.dma("sp", ds_c2, lambda e: e.dma_start(out=selo_sb, in_=selo))
            P.dma("sp", ds_c2, lambda e: e.dma_start(out=esm, in_=es_main))
            c3_all = P.dma("sp", ds_c2, lambda e: e.dma_start(out=est[0:8], in_=es_tail))
            qkv_s = [ar.alloc([3, 128], F32) for _ in range(3)]
            qb = [ar.alloc([128], BF16) for _ in range(3)]
            Qb_ring = Ring([ar.alloc([8, 128], F32) for _ in range(2)])
            kc_ring = Ring([ar.alloc([8, 128], F32) for _ in range(2)], P)
            vc_ring = Ring([ar.alloc([8, 128], F32) for _ in range(2)], P)
            prod = ar.alloc([8, 128], F32); S_ = ar.alloc([16], F32); Pm = ar.alloc([16], F32)
            WP_ring = Ring([ar.alloc([8, 130], BF16) for _ in range(2)])
            kn_ring = Ring([ar.alloc([128], F32) for _ in range(2)], P)
            vn_ring = Ring([ar.alloc([128], F32) for _ in range(2)], P)
            prod_t = ar.alloc([8, 128], F32); St = ar.alloc([16], F32); Pt = ar.alloc([16], F32)
            WPt_ring = Ring([ar.alloc([8, 130], BF16) for _ in range(2)])
            ofin = ar.alloc([130], F32); rl_ = ar.alloc([2], F32); atts = ar.alloc([128], BF16)
            ds_o3 = [P.new_dsem() for _ in range(3)]
            qsets = Ring([(psb[2], psb[3]), (psb[4], psb[5])])
            obank_free = [[], []]
            qkv_free = [[], [], []]
            th = lambda ap: ap.rearrange("p (t h) -> p t h", h=2)
            hd = lambda ap: ap.rearrange("p t (h d) -> p t h d", d=64)
            for p in range(8):
                out_acc = psb[6 + p % 2][0:NS, 0:130]
                first_mm = [True]
                evs = []
                new_qkv_free = [[], [], []]
                qbcs = []
                for g in range(3):
                    R = CACHE_ROWS[g]
                    ka, pacc, adeps = acc_ring.next()
                    for wi, col in enumerate((g * 1024 + p * 128, 3072 + g * 1024 + p * 128, 6144 + g * 1024 + p * 128)):
                        k, wb, wt = load_w(col)
                        for kc in range(16):
                            last = P.op("pe", lambda e, pacc=pacc, wi=wi, kc=kc, wb=wb: e.matmul(
                                pacc[0:NS, wi * 128:(wi + 1) * 128], lhsT=xnT[:, kc, NCTX:NCTX + NS], rhs=wb[:, kc, :], start=(kc == 0), stop=(kc == 15)),
                                deps=[wt, adeps, xn_all], signal=(kc == 15))
                        w_ring.release(k, [last])
                    ev_ = P.op("act", lambda e, pacc=pacc, g=g: e.activation(out=qkv_s[g][0:NS], in_=pacc[0:NS, 0:384].rearrange("p (a b) -> p a b", b=128), func=AF.Copy),
                               deps=[last, qkv_free[g]])
                    acc_ring.release(ka, [ev_])
                    evs.append(ev_)
                    qbc = P.op("pool", lambda e, g=g: e.tensor_copy(out=qb[g][0:NS], in_=qkv_s[g][0:NS, 0, :]), deps=[ev_, qkv_free[g]])
                    qbcs.append(qbc)
                    for b in range(4):
                        for wi, dst in ((1, oks[g]), (2, ovs[g])):
                            do = P.dma("sp", ds_o3[g], lambda e, dst=dst, b=b, R=R, p=p, g=g, wi=wi: e.dma_start(
                                out=dst[b, R - 8:R, p * 128:(p + 1) * 128], in_=qkv_s[g][8 * b:8 * b + 8, wi, :]), deps=[ev_])
                    new_qkv_free[g].append(do)
                mo = None
                for g in range(3):
                    dil = DILS[g]
                    nr = min(dil, 8)
                    for b in range(4):
                        ks, (pq0, pq1), qdeps = qsets.next()
                        mq3 = mq7 = None
                        for t in range(8):
                            bank = pq0 if t < 4 else pq1
                            tok = P.op("pe", lambda e, bank=bank, t=t, b=b, g=g: e.matmul(
                                bank[:, (t % 4) * 128:(t % 4 + 1) * 128], lhsT=selq_sb[0:NS, 8 * b + t, :], rhs=qb[g][0:NS, :], start=True, stop=True),
                                deps=[qbcs[g], qdeps, c3_all], signal=(t in (3, 7)))
                            if t == 3:
                                mq3 = tok
                            if t == 7:
                                mq7 = tok
                        kq, Qb, qbdeps = Qb_ring.next()
                        c0_ = P.op("dve", lambda e, Qb=Qb, pq0=pq0: e.tensor_copy(out=Qb[:, 0:4, :], in_=pq0[:, :].rearrange("p (a b) -> p a b", b=128)), deps=[mq3, qbdeps])
                        c1_ = P.op("act", lambda e, Qb=Qb, pq1=pq1: e.activation(out=Qb[:, 4:8, :], in_=pq1[:, :].rearrange("p (a b) -> p a b", b=128), func=AF.Copy), deps=[mq7, qbdeps])
                        qsets.release(ks, [c0_, c1_])
                        kk, Kc, kdeps = kc_ring.next()
                        lk = P.dma("sp", kc_ring.ds[kk], lambda e, Kc=Kc, g=g, b=b, p=p, dil=dil, nr=nr: e.dma_start(
                            out=Kc[:, 0:nr, :], in_=ck[g][b, :, p * 128:(p + 1) * 128].rearrange("(m r) c -> m r c", r=dil)[:, 0:nr, :]), deps=[kdeps])
                        kv_, Vc, vdeps = vc_ring.next()
                        lv = P.dma("sp", vc_ring.ds[kv_], lambda e, Vc=Vc, g=g, b=b, p=p, dil=dil, nr=nr: e.dma_start(
                            out=Vc[:, 0:nr, :], in_=cv[g][b, :, p * 128:(p + 1) * 128].rearrange("(m r) c -> m r c", r=dil)[:, 0:nr, :]), deps=[vdeps])
                        if g == 0:
                            p1 = P.op("dve", lambda e, Qb=Qb, Kc=Kc: e.tensor_tensor(out=prod, in0=Qb, in1=Kc[:, 0:1, :].broadcast_to([128, 8, 128]), op=ALU.mult), deps=[c0_, c1_, lk])
                        elif g == 2:
                            p1 = P.op("dve", lambda e, Qb=Qb, Kc=Kc: e.tensor_tensor(out=prod, in0=Qb, in1=Kc[:, 0:8, :], op=ALU.mult), deps=[c0_, c1_, lk])
                        else:
                            P.op("dve", lambda e, Qb=Qb, Kc=Kc: e.tensor_tensor(out=prod[:, 0:4, :], in0=Qb[:, 0:4, :], in1=Kc[:, 0:4, :], op=ALU.mult), deps=[c0_, c1_, lk])
                            p1 = P.op("dve", lambda e, Qb=Qb, Kc=Kc: e.tensor_tensor(out=prod[:, 4:8, :], in0=Qb[:, 4:8, :], in1=Kc[:, 0:4, :], op=ALU.mult), deps=[c0_, c1_, lk])
                        kc_ring.release(kk, [p1])
                        r_ = P.op("dve", lambda e: e.tensor_reduce(out=S_, in_=prod.rearrange("p t (h d) -> p (t h) d", d=64), axis=AX.X, op=ALU.add), deps=[p1])
                        ex = P.op("act", lambda e: e.activation(out=Pm, in_=S_, func=AF.Exp, scale=0.125), deps=[r_])
                        mk = P.op("dve", lambda e, g=g, p=p: e.tensor_tensor(out=th(Pm), in0=th(Pm), in1=esm[:, g, :, 2 * p:2 * p + 2], op=ALU.mult), deps=[ex, c3_all])
                        kw, WP, wdeps = WP_ring.next()
                        pm4 = lambda lo, hi: th(Pm)[:, lo:hi, :].unsqueeze(3).broadcast_to([128, hi - lo, 2, 64])
                        if g == 0:
                            w1 = P.op("dve", lambda e, WP=WP, Vc=Vc: e.tensor_tensor(out=hd(WP[:, :, 0:128]), in0=hd(Vc[:, 0:1, :].broadcast_to([128, 8, 128])), in1=pm4(0, 8), op=ALU.mult), deps=[mk, lv, wdeps])
                        elif g == 2:
                            w1 = P.op("dve", lambda e, WP=WP, Vc=Vc: e.tensor_tensor(out=hd(WP[:, :, 0:128]), in0=hd(Vc[:, 0:8, :]), in1=pm4(0, 8), op=ALU.mult), deps=[mk, lv, wdeps])
                        else:
                            P.op("dve", lambda e, WP=WP, Vc=Vc: e.tensor_tensor(out=hd(WP[:, 0:4, 0:128]), in0=hd(Vc[:, 0:4, :]), in1=pm4(0, 4), op=ALU.mult), deps=[mk, lv, wdeps])
                            w1 = P.op("dve", lambda e, WP=WP, Vc=Vc: e.tensor_tensor(out=hd(WP[:, 4:8, 0:128]), in0=hd(Vc[:, 0:4, :]), in1=pm4(4, 8), op=ALU.mult), deps=[mk, lv, wdeps])
                        w2 = P.op("dve", lambda e, WP=WP: e.tensor_copy(out=WP[:, :, 128:130], in_=th(Pm)), deps=[w1])
                        vc_ring.release(kv_, [w1])
                        for t in range(8):
                            j = 8 * b + t
                            mo = P.op("pe", lambda e, WP=WP, t=t, j=j, st=first_mm[0], out_acc=out_acc: e.matmul(out_acc, lhsT=selo_sb[:, 31 - j:63 - j], rhs=WP[:, t, :], start=st, stop=False),
                                      deps=[w2, obank_free[p % 2], c3_all], signal=(t == 7))
                            first_mm[0] = False
                        WP_ring.release(kw, [mo])
                        kn, knew, kndeps = kn_ring.next()
                        lkn = P.dma("sp", kn_ring.ds[kn], lambda e, knew=knew, g=g, b=b: e.dma_start(out=knew[0:8], in_=qkv_s[g][8 * b:8 * b + 8, 1, :]), deps=[evs[g], kndeps])
                        vn, vnew, vndeps = vn_ring.next()
                        lvn = P.dma("sp", vn_ring.ds[vn], lambda e, vnew=vnew, g=g, b=b: e.dma_start(out=vnew[0:8], in_=qkv_s[g][8 * b:8 * b + 8, 2, :]), deps=[evs[g], vndeps])
                        new_qkv_free[g] += [lkn, lvn]
                        pt1 = P.op("dve", lambda e, Qb=Qb, knew=knew: e.tensor_tensor(out=prod_t[0:8], in0=Qb[0:8], in1=knew[0:8].unsqueeze(1).broadcast_to([8, 8, 128]), op=ALU.mult), deps=[c0_, c1_, lkn])
                        Qb_ring.release(kq, [p1, pt1])
                        kn_ring.release(kn, [pt1])
                        rt = P.op("dve", lambda e: e.tensor_reduce(out=St[0:8], in_=prod_t[0:8].rearrange("p t (h d) -> p (t h) d", d=64), axis=AX.X, op=ALU.add), deps=[pt1])
                        ext = P.op("act", lambda e: e.activation(out=Pt[0:8], in_=St[0:8], func=AF.Exp, scale=0.125), deps=[rt])
                        mkt = P.op("dve", lambda e, g=g, p=p: e.tensor_tensor(out=th(Pt[0:8]), in0=th(Pt[0:8]), in1=est[0:8, g, :, 2 * p:2 * p + 2], op=ALU.mult), deps=[ext, c3_all])
                        kwt, WPt, wtdeps = WPt_ring.next()
                        wt1 = P.op("dve", lambda e, WPt=WPt, vnew=vnew: e.tensor_tensor(
                            out=hd(WPt[0:8, :, 0:128]), in0=vnew[0:8].rearrange("p (h d) -> p h d", d=64).unsqueeze(1).broadcast_to([8, 8, 2, 64]),
                            in1=th(Pt[0:8]).unsqueeze(3).broadcast_to([8, 8, 2, 64]), op=ALU.mult), deps=[mkt, lvn, wtdeps])
                        wt2 = P.op("dve", lambda e, WPt=WPt: e.tensor_copy(out=WPt[0:8, :, 128:130], in_=th(Pt[0:8])), deps=[wt1])
                        vn_ring.release(vn, [wt1])
                        lastu = (g == 2 and b == 3)
                        for t in range(8):
                            j = 8 * b + t
                            mo = P.op("pe", lambda e, WPt=WPt, t=t, j=j, sp_=(lastu and t == 7), out_acc=out_acc: e.matmul(out_acc, lhsT=selo_sb[0:8, 31 - j:63 - j], rhs=WPt[0:8, t, :], start=False, stop=sp_),
                                      deps=[wt2], signal=(t == 7))
                        WPt_ring.release(kwt, [mo])
                f1_ = P.op("dve", lambda e, out_acc=out_acc: e.tensor_copy(out=ofin[0:NS], in_=out_acc), deps=[mo])
                obank_free[p % 2] = [f1_]
                f2_ = P.op("dve", lambda e: e.reciprocal(out=rl_[0:NS], in_=ofin[0:NS, 128:130]), deps=[f1_])
                f3_ = P.op("dve", lambda e: e.tensor_tensor(out=atts[0:NS].rearrange("p (h d) -> p h d", d=64), in0=ofin[0:NS, 0:128].rearrange("p (h d) -> p h d", d=64),
                                                           in1=rl_[0:NS].unsqueeze(2).broadcast_to([NS, 2, 64]), op=ALU.mult), deps=[f2_])
                if 'dbg' in phases:
                    dd1 = P.dma("sp", ds_copy, lambda e, p=p: e.dma_start(out=dbg_ofin[p], in_=ofin[0:NS]), deps=[f3_])
                    dd2 = P.dma("sp", ds_copy, lambda e, p=p: e.dma_start(out=dbg_atts[p], in_=atts[0:NS]), deps=[f3_])
                    obank_free[p % 2] = [f1_, dd1, dd2]
                ka, pacc, adeps = acc_ring.next()
                pab = pacc[:].bitcast(BF16)[:, 0:NS]
                tr_ = P.op("pe", lambda e, pab=pab: e.transpose(out=pab, in_=atts[0:NS, :], identity=ident[0:NS, 0:NS]), deps=[f3_, adeps, c_id])
                cpa = P.op("act", lambda e, pab=pab, p=p: e.activation(out=attT[:, p, NOWN:NOWN + NS], in_=pab, func=AF.Copy), deps=[tr_])
                acc_ring.release(ka, [cpa])
                qkv_free = new_qkv_free
            P.barrier()

        if "B" in phases:
            ar.off = mark_persist
            P.nd = nd_persist
            gffn_bc = ar.alloc([D], F32); gfin_bc = ar.alloc([D], F32)
            kaT = ar.alloc([128], F32); kbT = ar.alloc([128], F32); sk_st = ar.alloc([128], F32)
            WB = 256
            wring = Ring([ar.alloc([16, WB], BF16) for _ in range(2)], P)
            gring = Ring([ar.alloc([D], BF16) for _ in range(6)], P)
            xt = ar.alloc([D], F32); xn2b = ar.alloc([D], BF16)
            h1s = [ar.alloc([D], F32) for _ in range(2)]; xn2s = [ar.alloc([D], F32) for _ in range(2)]
            eids = [ar.alloc([128], I32) for _ in range(2)]; gates = [ar.alloc([8, 16], F32) for _ in range(2)]
            accb = ar.alloc([D], F32); junkg = ar.alloc([D], BF16)
            attg = ar.alloc([16, 128], BF16); xn2T = ar.alloc([16, 128], BF16)
            sqb = xn2T
            qT = ar.alloc([16, 128], F32); sc = ar.alloc([16, 128], F32); scw = ar.alloc([128], F32)
            v16 = ar.alloc([16, 16], F32); i16 = ar.alloc([16, 16], U32); idxf = ar.alloc([16, 16], F32)
            cand = ar.alloc([256], F32); cid = ar.alloc([256], F32); cw = ar.alloc([256], F32); junk256 = ar.alloc([256], F32)
            iota_f = ar.alloc([256], F32); pos_u = ar.alloc([8, 16], U32); posf = ar.alloc([8, 16], F32)
            ts = ar.alloc([8, 16], F32); tsub = ar.alloc([8, 16], F32); ge = ar.alloc([8, 16], F32); zs = ar.alloc([8], F32)
            eidf = ar.alloc([128], F32)
            dots = ar.alloc([128], F32); actv = ar.alloc([128], F32); coef = ar.alloc([128], F32); st2 = ar.alloc([16], F32); st3 = ar.alloc([8], F32)
            eidP = ar.alloc([32], I32); gateP = ar.alloc([32], F32); sel4_sb = ar.alloc([32], F32)
            ds_b = P.new_dsem(); ds_y = P.new_dsem(); ds_p = P.new_dsem()
            P.dma("sp", ds_b, lambda e: e.dma_start(out=sel4_sb, in_=sel4_d))
            cb1 = P.dma("sp", ds_b, lambda e: e.dma_start(out=gffn_bc, in_=g_ffn.partition_broadcast(128)))
            P.dma("sp", ds_b, lambda e: e.dma_start(out=iota_f, in_=iota_d.partition_broadcast(128)))
            cb2 = P.dma("sp", ds_b, lambda e: e.dma_start(out=gfin_bc, in_=g_fin.partition_broadcast(128)))
            prevt = None
            for src, dst in ((ska, kaT), (skb, kbT)):
                l0 = P.dma("sp", ds_b, lambda e, src=src: e.dma_start(out=sk_st, in_=src), deps=[prevt])
                t0_ = P.op("pe", lambda e: e.transpose(out=psb[0][:, 0:128], in_=sk_st, identity=identf), deps=[l0, c_idf, prevt])
                prevt = P.op("act", lambda e, dst=dst: e.activation(out=dst, in_=psb[0][:, 0:128], func=AF.Copy), deps=[t0_])
            P.barrier()

            def load_wb(wsrc, cb):
                k, wb, fdeps = wring.next()
                t = P.dma("sp", wring.ds[k], lambda e, wb=wb, wsrc=wsrc, cb=cb: e.dma_start(
                    out=wb, in_=wsrc[:, cb * WB:(cb + 1) * WB].rearrange("(kc p) c -> p kc c", p=128)), deps=fdeps)
                return k, wb, t

            def rstd_of(ssq_ap, out_ap, dim, deps):
                a = P.op("act", lambda e: e.activation(out=out_ap, in_=ssq_ap, func=AF.Sqrt, scale=1.0 / dim, bias=epsc[0:out_ap.shape[0], 0:1]), deps=deps + [c_eps])
                return P.op("dve", lambda e: e.reciprocal(out=out_ap, in_=out_ap), deps=[a])

            ntiles = (9 if 'A3s' in phases else 8) if 'b1' not in phases else 1

            def front(i):
                n = 128 if i < 8 else NS
                c0 = i * 128
                h1 = h1s[i % 2]; xn2 = xn2s[i % 2]; eid = eids[i % 2]; gate = gates[i % 2]
                src = xp[NCTX - NOWN + c0:NCTX - NOWN + c0 + 128, :] if i < 8 else xs
                lx = P.dma("sp", ds_b, lambda e: e.dma_start(out=xt[0:n], in_=src))
                g1_ = P.op("pool", lambda e: e.tensor_tensor(out=attg[:, 0:8, 0:n], in0=attT[:, :, c0:c0 + n], in1=gatt.unsqueeze(2).broadcast_to([128, 8, n]), op=ALU.mult), deps=[c_gc])
                g2_ = P.op("pool", lambda e: e.tensor_tensor(out=attg[:, 8:16, 0:n], in0=convT[:, :, c0:c0 + n], in1=gconv.unsqueeze(2).broadcast_to([128, 8, n]), op=ALU.mult), deps=[c_gc])
                q1_ = P.op("pool", lambda e: e.tensor_tensor(out=sqb[:, 0:8, 0:n], in0=attT[:, :, c0:c0 + n], in1=attT[:, :, c0:c0 + n], op=ALU.mult))
                q2_ = P.op("pool", lambda e: e.tensor_tensor(out=sqb[:, 8:16, 0:n], in0=convT[:, :, c0:c0 + n], in1=convT[:, :, c0:c0 + n], op=ALU.mult))
                for part in range(2):
                    for kc in range(8):
                        mss = P.op("pe", lambda e, part=part, kc=kc: e.matmul(psb[0][0:n, part:part + 1], lhsT=sqb[:, part * 8 + kc, 0:n], rhs=ones_b[:, 0:1],
                                                                            start=(kc == 0), stop=(kc == 7)), deps=[q2_, c_ones], signal=(kc == 7 and part == 1))
                cps = P.op("dve", lambda e: e.tensor_copy(out=st2[0:n, 0:2], in_=psb[0][0:n, 0:2]), deps=[mss])
                r2_ = rstd_of(st2[0:n, 0:2], st2[0:n, 2:4], 1024, [cps])
                yield
                obank = Ring([(psb[2], psb[3]), (psb[4], psb[5])])
                hdone = None
                for cb in range(D // WB):
                    k, wb, wt = load_wb(wo_bf, cb)
                    ko, (pa, pc), odeps = obank.next()
                    for kc in range(16):
                        pb_ = pa if kc < 8 else pc
                        mo = P.op("pe", lambda e, pb_=pb_, kc=kc, wb=wb: e.matmul(pb_[0:n, 0:WB], lhsT=attg[:, kc, 0:n], rhs=wb[:, kc, :],
                                                                            start=(kc % 8 == 0), stop=(kc % 8 == 7)), deps=[wt, g2_, odeps], signal=(kc == 15))
                    wring.release(k, [mo])
                    cs_ = slice(cb * WB, (cb + 1) * WB)
                    e1_ = P.op("dve", lambda e, pa=pa, cs_=cs_: e.scalar_tensor_tensor(out=h1[0:n, cs_], in0=pa[0:n, 0:WB], scalar=st2[0:n, 2:3], in1=xt[0:n, cs_], op0=ALU.mult, op1=ALU.add), deps=[mo, r2_, lx])
                    hdone = P.op("dve", lambda e, pc=pc, cs_=cs_: e.scalar_tensor_tensor(out=h1[0:n, cs_], in0=pc[0:n, 0:WB], scalar=st2[0:n, 3:4], in1=h1[0:n, cs_], op0=ALU.mult, op1=ALU.add), deps=[e1_])
                    obank.release(ko, [hdone])
                    yield
                sq2 = P.op("act", lambda e: e.activation(out=xn2b[0:n], in_=h1[0:n], func=AF.Square, accum_out=st2[0:n, 4:5]), deps=[hdone])
                r3_ = rstd_of(st2[0:n, 4:5], st2[0:n, 5:6], D, [sq2])
                nm2 = P.op("dve", lambda e: e.scalar_tensor_tensor(out=xn2[0:n], in0=h1[0:n], scalar=st2[0:n, 5:6], in1=gffn_bc[0:n], op0=ALU.mult, op1=ALU.mult), deps=[r3_])
                nb2 = P.op("pool", lambda e: e.tensor_copy(out=xn2b[0:n], in_=xn2[0:n]), deps=[nm2])
                yield
                tbank = Ring([psb[0][:].bitcast(BF16), psb[1][:].bitcast(BF16)])
                xT_done = None
                for q4 in range(4):
                    kp, pst, pdeps = tbank.next()
                    for j in range(4):
                        kc = q4 * 4 + j
                        tt_ = P.op("pe", lambda e, pst=pst, kc=kc, j=j: e.transpose(out=pst[:, j * 128:j * 128 + n], in_=xn2b[0:n, kc * 128:(kc + 1) * 128],
                                                                               identity=ident[0:n, 0:n]), deps=[nb2, c_id, pdeps, cps], signal=(j == 3))
                    xT_done = P.op("act", lambda e, pst=pst, q4=q4: e.activation(out=xn2T[:, q4 * 4:q4 * 4 + 4, 0:n],
                                                                             in_=pst[:, 0:512].rearrange("p (a b) -> p a b", b=128)[:, :, 0:n], func=AF.Copy), deps=[tt_])
                    tbank.release(kp, [xT_done])
                    yield
                qbank = Ring([psb[2], psb[3], psb[4], psb[5]])
                q_done = None
                for cb in range(D // WB):
                    k, wb, wt = load_wb(wq_bf, cb)
                    for c4 in range(WB // 128):
                        cc = cb * (WB // 128) + c4
                        kq_, pq_, qdeps = qbank.next()
                        for kc in range(16):
                            mq = P.op("pe", lambda e, pq_=pq_, kc=kc, wb=wb, c4=c4: e.matmul(pq_[:, 0:n], lhsT=wb[:, kc, c4 * 128:(c4 + 1) * 128], rhs=xn2T[:, kc, 0:n],
                                                                                      start=(kc == 0), stop=(kc == 15)), deps=[wt, xT_done, qdeps, hdone], signal=(kc == 15))
                        q_done = P.op("act", lambda e, pq_=pq_, cc=cc: e.activation(out=qT[:, cc, 0:n], in_=pq_[:, 0:n], func=AF.Copy), deps=[mq])
                        qbank.release(kq_, [q_done])
                    wring.release(k, [mq])
                    yield
                sbank = Ring([psb[6], psb[7]])
                sc_done = None
                for c4g in range(4):
                    ksb, psc, sdeps = sbank.next()
                    for j in range(4):
                        cc = c4g * 4 + j
                        msc = P.op("pe", lambda e, psc=psc, cc=cc, j=j: e.matmul(psc[0:n, j * 128:(j + 1) * 128], lhsT=qT[:, cc, 0:n], rhs=(kaT if cc % 2 == 0 else kbT),
                                                                            start=True, stop=True), deps=[q_done, sdeps], signal=(j == 3))
                    sc_done = P.op("dve", lambda e, psc=psc, c4g=c4g: e.tensor_copy(out=sc[0:n, c4g * 4:c4g * 4 + 4, :], in_=psc[0:n, :].rearrange("p (a b) -> p a b", b=128)), deps=[msc])
                    sbank.release(ksb, [sc_done])
                    yield
                t_ = sc_done
                for cc in range(16):
                    t_ = P.op("dve", lambda e, cc=cc: e.max(out=v16[0:n, cc, 0:8], in_=sc[0:n, cc, :]), deps=[t_])
                    t_ = P.op("dve", lambda e, cc=cc: e.max_index(out=i16[0:n, cc, 0:8], in_max=v16[0:n, cc, 0:8], in_values=sc[0:n, cc, :]), deps=[t_])
                    t_ = P.op("dve", lambda e, cc=cc: e.match_replace(out=scw[0:n], in_to_replace=v16[0:n, cc, 0:8], in_values=sc[0:n, cc, :], imm_value=-1e30), deps=[t_])
                    t_ = P.op("dve", lambda e, cc=cc: e.max(out=v16[0:n, cc, 8:16], in_=scw[0:n]), deps=[t_])
                    t_ = P.op("dve", lambda e, cc=cc: e.max_index(out=i16[0:n, cc, 8:16], in_max=v16[0:n, cc, 8:16], in_values=scw[0:n]), deps=[t_])
                    yield
                t_ = P.op("dve", lambda e: e.tensor_copy(out=idxf[0:n], in_=i16[0:n]), deps=[t_])
                c3 = lambda ap: ap.rearrange("p (a b) -> p a b", b=16)
                for h in range(8):
                    t_ = P.op("dve", lambda e, h=h: e.tensor_tensor(out=c3(cand[0:n]), in0=v16[0:n, 2 * h, :].unsqueeze(2).broadcast_to([n, 16, 16]),
                                                                 in1=v16[0:n, 2 * h + 1, :].unsqueeze(1).broadcast_to([n, 16, 16]), op=ALU.add), deps=[t_])
                    t_ = P.op("dve", lambda e, h=h: e.scalar_tensor_tensor(out=c3(cid[0:n]), in0=idxf[0:n, 2 * h, :].unsqueeze(2).broadcast_to([n, 16, 16]), scalar=128.0,
                                                                        in1=idxf[0:n, 2 * h + 1, :].unsqueeze(1).broadcast_to([n, 16, 16]), op0=ALU.mult, op1=ALU.add), deps=[t_])
                    t_ = P.op("dve", lambda e, h=h: e.max(out=ts[0:n, h, 0:8], in_=cand[0:n]), deps=[t_])
                    t_ = P.op("dve", lambda e, h=h: e.max_index(out=pos_u[0:n, h, 0:8], in_max=ts[0:n, h, 0:8], in_values=cand[0:n]), deps=[t_])
                    t_ = P.op("dve", lambda e, h=h: e.match_replace(out=cw[0:n], in_to_replace=ts[0:n, h, 0:8], in_values=cand[0:n], imm_value=-1e30), deps=[t_])
                    t_ = P.op("dve", lambda e, h=h: e.max(out=ts[0:n, h, 8:16], in_=cw[0:n]), deps=[t_])
                    t_ = P.op("dve", lambda e, h=h: e.max_index(out=pos_u[0:n, h, 8:16], in_max=ts[0:n, h, 8:16], in_values=cw[0:n]), deps=[t_])
                    t_ = P.op("dve", lambda e, h=h: e.tensor_copy(out=posf[0:n, h, :], in_=pos_u[0:n, h, :]), deps=[t_])
                    yield
                    for k_ in range(16):
                        t_ = P.op("dve", lambda e, h=h, k_=k_: e.scalar_tensor_tensor(out=junk256[0:n], in0=iota_f[0:n], scalar=posf[0:n, h, k_:k_ + 1], in1=cid[0:n],
                                                                                   op0=ALU.is_equal, op1=ALU.mult, accum_out=eidf[0:n, h * 16 + k_:h * 16 + k_ + 1]), deps=[t_])
                        if k_ % 4 == 3:
                            yield
                t_ = P.op("dve", lambda e: e.tensor_scalar(out=eidf[0:n], in0=eidf[0:n], scalar1=0.0, scalar2=16383.0, op0=ALU.max, op1=ALU.min), deps=[t_])
                eid_done = P.op("dve", lambda e: e.tensor_copy(out=eid[0:n], in_=eidf[0:n]), deps=[t_])
                t_ = P.op("dve", lambda e: e.tensor_tensor(out=tsub[0:n], in0=ts[0:n], in1=ts[0:n, :, 0:1].broadcast_to([n, 8, 16]), op=ALU.subtract), deps=[eid_done])
                t_ = P.op("act", lambda e: e.activation(out=ge[0:n], in_=tsub[0:n], func=AF.Exp), deps=[t_])
                t_ = P.op("dve", lambda e: e.tensor_reduce(out=zs[0:n], in_=ge[0:n], axis=AX.X, op=ALU.add), deps=[t_])
                t_ = P.op("dve", lambda e: e.reciprocal(out=zs[0:n], in_=zs[0:n]), deps=[t_])
                P.op("dve", lambda e: e.tensor_tensor(out=gate[0:n], in0=ge[0:n], in1=zs[0:n].unsqueeze(2).broadcast_to([n, 8, 16]), op=ALU.mult), deps=[t_])
                yield

            def gather(i):
                n = 128 if i < 8 else NS
                c0 = i * 128
                h1 = h1s[i % 2]; xn2 = xn2s[i % 2]; eid = eids[i % 2]; gate = gates[i % 2]
                dst = yp[c0:c0 + 128, :] if i < 8 else ys
                gflat = gate.rearrange("p a b -> p (a b)")
                d_ = None
                for s_ in range(128):
                    k, ub, gdeps = gring.next()
                    gt_ = P.dma("pool", gring.ds[k], lambda e, ub=ub, s_=s_: e.indirect_dma_start(
                        out=ub[0:n], out_offset=None, in_=eu_bf, in_offset=bass.IndirectOffsetOnAxis(ap=eid[0:n, s_:s_ + 1], axis=0)), deps=[gdeps])
                    d_ = P.op("dve", lambda e, ub=ub, s_=s_: e.scalar_tensor_tensor(out=junkg[0:n], in0=ub[0:n], scalar=1.0, in1=xn2[0:n], op0=ALU.mult, op1=ALU.mult,
                                                                              accum_out=dots[0:n, s_:s_ + 1]), deps=[gt_])
                    gring.release(k, [d_])
                    if s_ % 4 == 3:
                        yield
                a_ = P.op("act", lambda e: e.activation(out=actv[0:n], in_=dots[0:n], func=AF.Gelu), deps=[d_])
                cf = P.op("dve", lambda e: e.tensor_tensor(out=coef[0:n], in0=actv[0:n], in1=gflat[0:n], op=ALU.mult), deps=[a_])
                for s_ in range(128):
                    k, vb, gdeps = gring.next()
                    gt_ = P.dma("pool", gring.ds[k], lambda e, vb=vb, s_=s_: e.indirect_dma_start(
                        out=vb[0:n], out_offset=None, in_=ev_bf, in_offset=bass.IndirectOffsetOnAxis(ap=eid[0:n, s_:s_ + 1], axis=0)), deps=[gdeps])
                    if s_ == 0:
                        d_ = P.op("dve", lambda e, vb=vb: e.tensor_scalar(out=accb[0:n], in0=vb[0:n], scalar1=coef[0:n, 0:1], scalar2=None, op0=ALU.mult), deps=[gt_, cf])
                    else:
                        d_ = P.op("dve", lambda e, vb=vb, s_=s_: e.scalar_tensor_tensor(out=accb[0:n], in0=vb[0:n], scalar=coef[0:n, s_:s_ + 1], in1=accb[0:n],
                                                                                  op0=ALU.mult, op1=ALU.add), deps=[gt_])
                    gring.release(k, [d_])
                    if s_ % 4 == 3:
                        yield
                f_ = P.op("dve", lambda e: e.tensor_tensor(out=accb[0:n], in0=accb[0:n], in1=h1[0:n], op=ALU.add), deps=[d_])
                sq3 = P.op("act", lambda e: e.activation(out=junkg[0:n], in_=accb[0:n], func=AF.Square, accum_out=st3[0:n, 0:1]), deps=[f_])
                r4_ = rstd_of(st3[0:n, 0:1], st3[0:n, 1:2], D, [sq3])
                y_ = P.op("dve", lambda e: e.scalar_tensor_tensor(out=accb[0:n], in0=accb[0:n], scalar=st3[0:n, 1:2], in1=gfin_bc[0:n], op0=ALU.mult, op1=ALU.mult), deps=[r4_])
                P.dma("sp", ds_y, lambda e: e.dma_start(out=dst, in_=accb[0:n]), deps=[y_])
                yield

            def gather_packed(i):
                n = NS
                h1 = h1s[i % 2]; xn2 = xn2s[i % 2]; eid = eids[i % 2]; gate = gates[i % 2]
                xn2P = xn2s[(i + 1) % 2]
                gflat = gate.rearrange("p a b -> p (a b)")
                mv = None
                for q in range(4):
                    P.dma("sp", ds_p, lambda e, q=q: e.dma_start(out=eidP[32 * q:32 * q + 32, :], in_=eid[0:n, 32 * q:32 * q + 32]))
                    P.dma("sp", ds_p, lambda e, q=q: e.dma_start(out=gateP[32 * q:32 * q + 32, :], in_=gflat[0:n, 32 * q:32 * q + 32]))
                    mv = P.dma("sp", ds_p, lambda e, q=q: e.dma_start(out=xn2P[32 * q:32 * q + 32, :], in_=xn2[0:n, :]))
                d_ = None
                for j in range(32):
                    k, ub, gdeps = gring.next()
                    gt_ = P.dma("pool", gring.ds[k], lambda e, ub=ub, j=j: e.indirect_dma_start(
                        out=ub, out_offset=None, in_=eu_bf, in_offset=bass.IndirectOffsetOnAxis(ap=eidP[:, j:j + 1], axis=0)), deps=[gdeps, mv])
                    d_ = P.op("dve", lambda e, ub=ub, j=j: e.scalar_tensor_tensor(out=junkg, in0=ub, scalar=1.0, in1=xn2P, op0=ALU.mult, op1=ALU.mult,
                                                                            accum_out=dots[:, j:j + 1]), deps=[gt_, mv])
                    gring.release(k, [d_])
                    if j % 4 == 3:
                        yield
                a_ = P.op("act", lambda e: e.activation(out=actv[:, 0:32], in_=dots[:, 0:32], func=AF.Gelu), deps=[d_])
                cf = P.op("dve", lambda e: e.tensor_tensor(out=coef[:, 0:32], in0=actv[:, 0:32], in1=gateP, op=ALU.mult), deps=[a_, mv])
                for j in range(32):
                    k, vb, gdeps = gring.next()
                    gt_ = P.dma("pool", gring.ds[k], lambda e, vb=vb, j=j: e.indirect_dma_start(
                        out=vb, out_offset=None, in_=ev_bf, in_offset=bass.IndirectOffsetOnAxis(ap=eidP[:, j:j + 1], axis=0)), deps=[gdeps, mv])
                    if j == 0:
                        d_ = P.op("dve", lambda e, vb=vb: e.tensor_scalar(out=accb, in0=vb, scalar1=coef[:, 0:1], scalar2=None, op0=ALU.mult), deps=[gt_, cf])
                    else:
                        d_ = P.op("dve", lambda e, vb=vb, j=j: e.scalar_tensor_tensor(out=accb, in0=vb, scalar=coef[:, j:j + 1], in1=accb, op0=ALU.mult, op1=ALU.add), deps=[gt_])
                    gring.release(k, [d_])
                    if j % 4 == 3:
                        yield
                fin = None
                for cbk in range(4):
                    mm_ = P.op("pe", lambda e, cbk=cbk: e.matmul(psb[2 + cbk][0:n, :], lhsT=sel4_sb, rhs=accb[:, cbk * 512:(cbk + 1) * 512], start=True, stop=True), deps=[d_])
                    fin = P.op("dve", lambda e, cbk=cbk: e.tensor_tensor(out=xt[0:n, cbk * 512:(cbk + 1) * 512], in0=psb[2 + cbk][0:n, :], in1=h1[0:n, cbk * 512:(cbk + 1) * 512], op=ALU.add), deps=[mm_])
                sq3 = P.op("act", lambda e: e.activation(out=junkg[0:n], in_=xt[0:n], func=AF.Square, accum_out=st3[0:n, 0:1]), deps=[fin])
                r4_ = rstd_of(st3[0:n, 0:1], st3[0:n, 1:2], D, [sq3])
                y_ = P.op("dve", lambda e: e.scalar_tensor_tensor(out=xt[0:n], in0=xt[0:n], scalar=st3[0:n, 1:2], in1=gfin_bc[0:n], op0=ALU.mult, op1=ALU.mult), deps=[r4_])
                P.dma("sp", ds_y, lambda e: e.dma_start(out=ys, in_=xt[0:n]), deps=[y_])
                yield

            for _ in front(0):
                pass
            P.barrier()
            for i in range(ntiles):
                gens = [gather(i) if i < 8 else gather_packed(i)] + ([front(i + 1)] if i + 1 < ntiles else [])
                while gens:
                    for gph in list(gens):
                        try:
                            next(gph)
                        except StopIteration:
                            gens.remove(gph)
                P.barrier()

        if 'dbg' in phases:
            P.dma("sp", ds_copy, lambda e: e.dma_start(out=dbg_att, in_=attT.rearrange("p a b -> p (a b)")))
            P.dma("sp", ds_copy, lambda e: e.dma_start(out=dbg_conv, in_=convT.rearrange("p a b -> p (a b)")))
        P.barrier()
        with nc.Block() as block:
            P.replay(block)
    return nc


def _alibi():
    n = 48
    i = np.arange(1, n + 1, dtype=np.float32)
    return np.exp2(-8.0 * i / n).astype(np.float32).reshape(3, 16)


def _etables(half):
    sl = _alibi()
    k = np.arange(128)[:, None]; q = np.arange(128)[None, :]
    out = []
    for g, dil in ((0, 1), (1, 4)):
        e = np.zeros((128, 16, 3, 128), np.float32)
        for h in range(16):
            s = sl[g, h]
            steps_prev = q - k + 128
            prev = np.where(steps_prev <= 128, np.exp(-s * (steps_prev * dil).astype(np.float32)), 0.0)
            steps_cur = q - k
            cur = np.where(steps_cur >= 0, np.exp(-s * (steps_cur * dil).astype(np.float32)), 0.0)
            e[:, h, 0] = prev if half == 1 else 0.0
            e[:, h, 1] = prev
            e[:, h, 2] = cur
        out.append(e.astype(ml_dtypes.bfloat16))
    e2 = np.zeros((128, 16, 64), np.float32)
    i = np.arange(128)[:, None]; j = np.arange(64)[None, :]
    for h in range(16):
        s = sl[2, h]
        if half == 1:
            steps = 64 + j - i
            e2[:, h] = np.where(steps >= 0, np.exp(-s * (steps * 16).astype(np.float32)), 0.0)
        else:
            steps = j - (i - 64)
            e2[:, h] = np.where((steps >= 0) & (i >= 64), np.exp(-s * (steps * 16).astype(np.float32)), 0.0)
    out.append(e2.astype(ml_dtypes.bfloat16))
    return out


def _sample_tables():
    sl = _alibi()
    es_main = np.zeros((128, 3, 8, 16), np.float32)
    es_tail = np.zeros((8, 3, 8, 16), np.float32)
    for g, dil in enumerate(DILS):
        n_prev = CACHE_ROWS[g]
        for t in range(8):
            r = t % dil
            for m in range(128):
                row = dil * m + r
                d = n_prev + t - row
                st = d // dil
                if d % dil == 0 and 0 <= st <= 128 and r < 8:
                    es_main[m, g, t] = np.exp(-sl[g] * np.float32(d))
            for tp in range(8):
                d = t - tp
                if d >= 0 and d % dil == 0 and d // dil <= 128:
                    es_tail[tp, g, t] = np.exp(-sl[g] * np.float32(d))
    return es_main, es_tail


_NC_CACHE = {}


def kernel(x_prompt, x_sample, cache_k_g0, cache_v_g0, cache_k_g1, cache_v_g1, cache_k_g2, cache_v_g2,
           state_conv, norm_mix, w_in, conv_w, norm_att_out, norm_conv_out, w_out, norm_ffn, w_pq,
           sub_keys_a, sub_keys_b, expert_u, expert_v, norm_final, _phases=PHASES):
    f = lambda a: np.ascontiguousarray(np.asarray(a, dtype=np.float32))
    x_prompt = f(x_prompt); x_sample = f(x_sample)
    cks = [f(cache_k_g0), f(cache_k_g1), f(cache_k_g2)]
    cvs = [f(cache_v_g0), f(cache_v_g1), f(cache_v_g2)]
    state_conv = f(state_conv)
    shared = dict(w_in=f(w_in)[0], conv_w=f(conv_w)[0], g_mix=f(norm_mix)[0], g_att=f(norm_att_out)[0],
                  g_conv=f(norm_conv_out)[0], w_out=f(w_out)[0], g_ffn=f(norm_ffn)[0], w_pq=f(w_pq)[0],
                  ska=f(sub_keys_a)[0], skb=f(sub_keys_b)[0], eu=f(expert_u)[0], ev=f(expert_v)[0], g_fin=f(norm_final),
                  ident_b=np.eye(128, dtype=np.float32).astype(ml_dtypes.bfloat16), ident_f=np.eye(128, dtype=np.float32))
    es_main, es_tail = _sample_tables()
    shared["es_main"] = es_main; shared["es_tail"] = es_tail
    selq = np.zeros((32, 32, 128), np.float32)
    for j in range(32):
        selq[j, j, :] = 1.0
    shared["selq"] = selq.astype(ml_dtypes.bfloat16)
    selo = np.zeros((128, 64), np.float32); selo[:, 31] = 1.0
    shared["selo"] = selo.astype(ml_dtypes.bfloat16)
    shared["iota256"] = np.arange(256, dtype=np.float32)
    sel4 = np.zeros((128, 32), np.float32)
    sel4[np.arange(128), np.arange(128) % 32] = 1.0
    shared["sel4"] = sel4
    et = [_etables(0), _etables(1)]
    in_maps = []
    for c in range(8):
        b, half = c // 2, c % 2
        if half == 1:
            xp = x_prompt[b]
        else:
            xp = np.concatenate([np.zeros((NOWN, D), np.float32), x_prompt[b, 0:NOWN]], axis=0)
        m = dict(shared)
        m["xp"] = np.ascontiguousarray(xp)
        m["xs"] = np.ascontiguousarray(x_sample[4 * c:4 * c + 4].reshape(NS, D))
        for g in range(3):
            m["ck%d" % g] = np.ascontiguousarray(cks[g][0, 4 * c:4 * c + 4].reshape(4, CACHE_ROWS[g], 1024))
            m["cv%d" % g] = np.ascontiguousarray(cvs[g][0, 4 * c:4 * c + 4].reshape(4, CACHE_ROWS[g], 1024))
        m["sconv"] = np.ascontiguousarray(state_conv[0, 4 * c:4 * c + 4])
        m["e0"], m["e1"], m["e2"] = et[half]
        in_maps.append(m)
    key = tuple(_phases)
    if key not in _NC_CACHE:
        _NC_CACHE[key] = build(_phases)
    nc = _NC_CACHE[key]
    res = run_bass_kernel_spmd(nc, in_maps, core_ids=list(range(8))).results
    y_prompt = np.zeros((4, 2048, D), np.float32)
    y_sample = np.zeros((32, 8, D), np.float32)
    nk_p = [np.zeros((1, 4, HALO[g] if g < 2 else 2048, 16, 64), np.float32) for g in range(3)]
    nv_p = [np.zeros((1, 4, HALO[g] if g < 2 else 2048, 16, 64), np.float32) for g in range(3)]
    nconv_p = np.zeros((1, 4, 2, 1024), np.float32)
    nk_s = [np.zeros((1, 32, CACHE_ROWS[g], 16, 64), np.float32) for g in range(3)]
    nv_s = [np.zeros((1, 32, CACHE_ROWS[g], 16, 64), np.float32) for g in range(3)]
    nconv_s = np.zeros((1, 32, 2, 1024), np.float32)
    for c in range(8):
        b, half = c // 2, c % 2
        r = res[c]
        y_prompt[b, half * NOWN:(half + 1) * NOWN] = r["yp"]
        y_sample[4 * c:4 * c + 4] = r["ys"].reshape(4, 8, D)
        if half == 1:
            for g in range(2):
                nk_p[g][0, b] = r["okp%d" % g].reshape(HALO[g], 16, 64)
                nv_p[g][0, b] = r["ovp%d" % g].reshape(HALO[g], 16, 64)
            nconv_p[0, b] = r["oconvp"]
        nk_p[2][0, b, half * NOWN:(half + 1) * NOWN] = r["okp2"].reshape(NOWN, 16, 64)
        nv_p[2][0, b, half * NOWN:(half + 1) * NOWN] = r["ovp2"].reshape(NOWN, 16, 64)
        for g in range(3):
            nk_s[g][0, 4 * c:4 * c + 4] = r["oks%d" % g].reshape(4, CACHE_ROWS[g], 16, 64)
            nv_s[g][0, 4 * c:4 * c + 4] = r["ovs%d" % g].reshape(4, CACHE_ROWS[g], 16, 64)
        nconv_s[0, 4 * c:4 * c + 4] = r["oconvs"]
    return (y_prompt, y_sample, nk_p[0], nv_p[0], nk_p[1], nv_p[1], nk_p[2], nv_p[2], nconv_p,
            nk_s[0], nv_s[0], nk_s[1], nv_s[1], nk_s[2], nv_s[2], nconv_s)
```
